# Optimizing a Trainium2 kernel written in Bass

```python
import jax
import jax.numpy as jnp
from jax import lax
import numpy as np

D_MODEL = 1024
BATCH = 8
SEQ = 4096
DEPTH = 2

HEAD_DIM = 64
RET_W = 3 * D_MODEL // 8
RET_HEADS = RET_W // HEAD_DIM
HGRN_W = 3 * D_MODEL // 8
HGRN_HEADS = HGRN_W // HEAD_DIM
HGRN_DK = HEAD_DIM
GLA_W = D_MODEL // 4
GLA_HEADS = GLA_W // HEAD_DIM
GLA_DK = HEAD_DIM // 2
GLA_QK = GLA_HEADS * GLA_DK
GLA_GATE_RANK = 16
GLA_TAU = 16.0
D_MIX = RET_W + HGRN_W + GLA_W
CHUNK = 64
ROPE_BASE = 10000.0
EPS = 1e-6
F_MIN = 1e-30
N_GROUPS = 4
EXPERTS_PER_GROUP = 8
N_EXPERTS = N_GROUPS * EXPERTS_PER_GROUP
TOP_K_INNER = 2
D_EXPERT = D_MODEL // 4
N_MOD = 6
IN_PROJ_SIZES = (RET_W,) * 4 + (HGRN_W,) * 4 + (GLA_QK, GLA_QK, GLA_W, GLA_W, GLA_GATE_RANK)
IN_PROJ_DIM = sum(IN_PROJ_SIZES)

kernel_name = "hybrid_ret_hgrn2_gla_hmoe_adaln"


def rmsnorm(x, gain):
    xf = x.astype(jnp.float32)
    y = xf * lax.rsqrt(jnp.mean(xf * xf, axis=-1, keepdims=True) + EPS)
    return y.astype(x.dtype) * gain


def split_heads(t, n_heads):
    b, s, w = t.shape
    return t.reshape(b, s, n_heads, w // n_heads).transpose(0, 2, 1, 3).astype(jnp.float32)


def head_rmsnorm(o, gain):
    b, h, s, d = o.shape
    o = o.transpose(0, 2, 1, 3)
    o = o * lax.rsqrt(jnp.mean(o * o, axis=-1, keepdims=True) + EPS)
    return o.reshape(b, s, h * d) * gain.astype(jnp.float32)


def rope_tables(positions, dim):
    inv_freq = ROPE_BASE ** (-jnp.arange(0, dim, 2, dtype=jnp.float32) / dim)
    ang = positions.astype(jnp.float32)[..., None] * inv_freq
    return jnp.cos(ang)[:, None], jnp.sin(ang)[:, None]


def apply_rope(t, cos, sin):
    t1, t2 = jnp.split(t, 2, axis=-1)
    return jnp.concatenate([t1 * cos - t2 * sin, t1 * sin + t2 * cos], axis=-1)


def retention_chunkwise(q, k, v):
    b, h, s, dk = q.shape
    dv = v.shape[-1]
    n = s // CHUNK
    log_gamma = jnp.log1p(-jnp.exp2(-5.0 - jnp.arange(h, dtype=jnp.float32)))
    idx = jnp.arange(CHUNK, dtype=jnp.float32)
    rel = idx[:, None] - idx[None, :]
    decay = jnp.where(rel >= 0, jnp.exp(log_gamma[:, None, None] * jnp.maximum(rel, 0.0)), 0.0)
    zeta = jnp.exp(log_gamma[:, None] * (CHUNK - 1 - idx))
    xi = jnp.exp(log_gamma[:, None] * (idx + 1.0))
    chunk_decay = jnp.exp(log_gamma * CHUNK)[:, None, None]
    qc = q.reshape(b, h, n, CHUNK, dk)
    kc = k.reshape(b, h, n, CHUNK, dk)
    vc = v.reshape(b, h, n, CHUNK, dv)
    scores = jnp.einsum('bhnid,bhnjd->bhnij', qc, kc) * decay[:, None]
    o_intra = jnp.einsum('bhnij,bhnje->bhnie', scores, vc)
    u = jnp.einsum('bhnjd,hj,bhnje->nbhde', kc, zeta, vc)

    def step(r, u_i):
        return r * chunk_decay + u_i, r

    _, r_prev = lax.scan(step, jnp.zeros((b, h, dk, dv), jnp.float32), u)
    o_inter = jnp.einsum('bhnid,nbhde,hi->bhnie', qc, r_prev, xi)
    return (o_intra + o_inter).reshape(b, h, s, dv)


def gated_linear_recurrence_chunkwise(q, k, v, log_a):
    b, h, s, dk = q.shape
    dv = v.shape[-1]
    n = s // CHUNK
    mask = jnp.tril(jnp.ones((CHUNK, CHUNK), dtype=bool))[:, :, None]

    def to_chunks(t):
        return jnp.moveaxis(t.reshape(b, h, n, CHUNK, t.shape[-1]), 2, 0)

    def step(state, inp):
        qc, kc, vc, lac = inp
        cum = jnp.cumsum(lac, axis=-2)
        o_inter = jnp.einsum('bhcd,bhde->bhce', qc * jnp.exp(cum), state)
        diff = cum[:, :, :, None, :] - cum[:, :, None, :, :]
        dec = jnp.where(mask, jnp.exp(jnp.where(mask, diff, 0.0)), 0.0)
        scores = jnp.einsum('bhtd,bhsd,bhtsd->bhts', qc, kc, dec)
        o = o_inter + jnp.einsum('bhts,bhse->bhte', scores, vc)
        cum_last = cum[:, :, -1:, :]
        new_state = (jnp.exp(cum_last[:, :, 0, :])[..., None] * state
                     + jnp.einsum('bhsd,bhse->bhde', kc * jnp.exp(cum_last - cum), vc))
        return new_state, o

    s0 = jnp.zeros((b, h, dk, dv), jnp.float32)
    _, o = lax.scan(step, s0, (to_chunks(q), to_chunks(k), to_chunks(v), to_chunks(log_a)))
    return jnp.moveaxis(o, 0, 2).reshape(b, h, s, dv)


def hybrid_mixer(h, cos, sin, w_in, ret_norm, hgrn_norm, hgrn_lb, gla_wa2, gla_ba, gla_norm, w_out):
    proj = h @ w_in
    splits = np.cumsum(IN_PROJ_SIZES)[:-1].tolist()
    (rq, rk, rv, rg, hq, hf, hi, hg, gq, gk, gv, gg, ga) = jnp.split(proj, splits, axis=-1)

    q = apply_rope(split_heads(rq, RET_HEADS), cos, sin)
    k = apply_rope(split_heads(rk, RET_HEADS), cos, sin) * HEAD_DIM ** -0.5
    o_ret = retention_chunkwise(q, k, split_heads(rv, RET_HEADS))
    o_ret = head_rmsnorm(o_ret, ret_norm) * jax.nn.silu(rg.astype(jnp.float32))

    lb = hgrn_lb.astype(jnp.float32).reshape(HGRN_HEADS, 1, HGRN_DK)
    z = split_heads(hf, HGRN_HEADS)
    f = lb + (1.0 - lb) * jax.nn.sigmoid(z)
    log_f = jnp.log(jnp.maximum(f, F_MIN))
    k = (1.0 - lb) * jax.nn.sigmoid(-z)
    q = jax.nn.silu(split_heads(hq, HGRN_HEADS)) * HGRN_DK ** -0.5
    o_hgrn = gated_linear_recurrence_chunkwise(q, k, split_heads(hi, HGRN_HEADS), log_f)
    o_hgrn = head_rmsnorm(o_hgrn, hgrn_norm) * jax.nn.silu(hg.astype(jnp.float32))

    log_a = jax.nn.log_sigmoid((ga @ gla_wa2 + gla_ba).astype(jnp.float32)) / GLA_TAU
    q = split_heads(gq, GLA_HEADS) * GLA_DK ** -0.5
    o_gla = gated_linear_recurrence_chunkwise(q, split_heads(gk, GLA_HEADS), split_heads(gv, GLA_HEADS),
                                              split_heads(log_a, GLA_HEADS))
    o_gla = head_rmsnorm(o_gla, gla_norm) * jax.nn.silu(gg.astype(jnp.float32))

    merged = jnp.concatenate([o_ret, o_hgrn, o_gla], axis=-1).astype(h.dtype)
    return merged @ w_out


def hierarchical_moe(h, w_rg, b_rg, w_re, b_re, w_gate, w_up, w_down):
    b, s, d = h.shape
    tok = h.reshape(b * s, d)
    g_logits = (tok @ w_rg + b_rg).astype(jnp.float32)
    g_idx = jnp.argmax(g_logits, axis=-1)
    g_w = jnp.max(jax.nn.softmax(g_logits, axis=-1), axis=-1, keepdims=True)
    e_logits = (tok @ w_re + b_re).astype(jnp.float32).reshape(-1, N_GROUPS, EXPERTS_PER_GROUP)
    e_in_group = jnp.einsum('nge,ng->ne', e_logits, jax.nn.one_hot(g_idx, N_GROUPS, dtype=jnp.float32))
    top_val, top_idx = lax.top_k(e_in_group, TOP_K_INNER)
    top_w = jax.nn.softmax(top_val, axis=-1) * g_w
    expert_ids = g_idx[:, None] * EXPERTS_PER_GROUP + top_idx
    combine = jnp.einsum('nk,nke->ne', top_w,
                         jax.nn.one_hot(expert_ids, N_EXPERTS, dtype=jnp.float32)).astype(h.dtype)
    out = jnp.zeros_like(tok)
    for e in range(N_EXPERTS):
        hid = jax.nn.silu(tok @ w_gate[e]) * (tok @ w_up[e])
        out = out + combine[:, e:e + 1] * (hid @ w_down[e])
    return out.reshape(b, s, d)


def setup_inputs(seed: int = 0) -> dict:
    key = jax.random.key(seed)
    ks = jax.random.split(key, 24)
    f32 = jnp.float32

    def nrm(k, shape, scale):
        return jax.random.normal(k, shape, f32) * scale

    x = nrm(ks[0], (BATCH, SEQ, D_MODEL), 1.0)
    c = nrm(ks[1], (BATCH, D_MODEL), 1.0)
    positions = (jax.random.randint(ks[2], (BATCH, 1), 0, 1024)
                 + jnp.arange(SEQ, dtype=jnp.int32)[None, :]).astype(jnp.int32)
    w_ada = nrm(ks[3], (DEPTH, D_MODEL, N_MOD * D_MODEL), 0.5 * D_MODEL ** -0.5)
    b_ada = nrm(ks[4], (DEPTH, N_MOD * D_MODEL), 0.02)
    norm_mix = 1.0 + nrm(ks[5], (DEPTH, D_MODEL), 0.02)
    norm_ffn = 1.0 + nrm(ks[6], (DEPTH, D_MODEL), 0.02)
    w_in = nrm(ks[7], (DEPTH, D_MODEL, IN_PROJ_DIM), D_MODEL ** -0.5)
    ret_norm = 1.0 + nrm(ks[8], (DEPTH, RET_W), 0.02)
    hgrn_norm = 1.0 + nrm(ks[9], (DEPTH, HGRN_W), 0.02)
    hgrn_lb_logits = nrm(ks[10], (DEPTH, HGRN_HEADS * HGRN_DK), 1.0)
    gla_wa2 = nrm(ks[11], (DEPTH, GLA_GATE_RANK, GLA_QK), GLA_GATE_RANK ** -0.5)
    gla_ba = nrm(ks[12], (DEPTH, GLA_QK), 0.1)
    gla_norm = 1.0 + nrm(ks[13], (DEPTH, GLA_W), 0.02)
    w_out = nrm(ks[14], (DEPTH, D_MIX, D_MODEL), D_MIX ** -0.5)
    router_group_w = nrm(ks[15], (DEPTH, D_MODEL, N_GROUPS), D_MODEL ** -0.5)
    router_group_b = nrm(ks[16], (DEPTH, N_GROUPS), 0.01)
    router_expert_w = nrm(ks[17], (DEPTH, D_MODEL, N_EXPERTS), D_MODEL ** -0.5)
    router_expert_b = nrm(ks[18], (DEPTH, N_EXPERTS), 0.01)
    expert_w_gate = nrm(ks[19], (DEPTH, N_EXPERTS, D_MODEL, D_EXPERT), D_MODEL ** -0.5)
    expert_w_up = nrm(ks[20], (DEPTH, N_EXPERTS, D_MODEL, D_EXPERT), D_MODEL ** -0.5)
    expert_w_down = nrm(ks[21], (DEPTH, N_EXPERTS, D_EXPERT, D_MODEL), D_EXPERT ** -0.5)
    norm_final = 1.0 + nrm(ks[22], (D_MODEL,), 0.02)
    return {"x": x, "c": c, "positions": positions, "w_ada": w_ada, "b_ada": b_ada,
            "norm_mix": norm_mix, "norm_ffn": norm_ffn, "w_in": w_in, "ret_norm": ret_norm,
            "hgrn_norm": hgrn_norm, "hgrn_lb_logits": hgrn_lb_logits, "gla_wa2": gla_wa2,
            "gla_ba": gla_ba, "gla_norm": gla_norm, "w_out": w_out,
            "router_group_w": router_group_w, "router_group_b": router_group_b,
            "router_expert_w": router_expert_w, "router_expert_b": router_expert_b,
            "expert_w_gate": expert_w_gate, "expert_w_up": expert_w_up,
            "expert_w_down": expert_w_down, "norm_final": norm_final}


def reference(x, c, positions, w_ada, b_ada, norm_mix, norm_ffn, w_in, ret_norm, hgrn_norm,
              hgrn_lb_logits, gla_wa2, gla_ba, gla_norm, w_out, router_group_w, router_group_b,
              router_expert_w, router_expert_b, expert_w_gate, expert_w_up, expert_w_down,
              norm_final):
    cos, sin = rope_tables(positions, HEAD_DIM)
    lb_w = jax.nn.softmax(hgrn_lb_logits.astype(jnp.float32), axis=0)
    lower_bounds = jnp.cumsum(lb_w, axis=0) - lb_w[0]
    c_act = jax.nn.silu(c)
    for layer in range(DEPTH):
        mod = c_act @ w_ada[layer] + b_ada[layer]
        shift_m, scale_m, gate_m, shift_f, scale_f, gate_f = [m[:, None, :] for m in jnp.split(mod, N_MOD, axis=-1)]
        h = rmsnorm(x, norm_mix[layer]) * (1.0 + scale_m) + shift_m
        x = x + gate_m * hybrid_mixer(h, cos, sin, w_in[layer], ret_norm[layer], hgrn_norm[layer],
                                      lower_bounds[layer], gla_wa2[layer], gla_ba[layer],
                                      gla_norm[layer], w_out[layer])
        h = rmsnorm(x, norm_ffn[layer]) * (1.0 + scale_f) + shift_f
        x = x + gate_f * hierarchical_moe(h, router_group_w[layer], router_group_b[layer],
                                          router_expert_w[layer], router_expert_b[layer],
                                          expert_w_gate[layer], expert_w_up[layer], expert_w_down[layer])
    return rmsnorm(x, norm_final)
```

```python
import contextlib
import math
import numpy as np
import ml_dtypes
import concourse.bass as bass
import concourse.mybir as mybir
from concourse.bass_utils import run_bass_kernel_spmd

F32 = mybir.dt.float32
BF16 = mybir.dt.bfloat16
I32 = mybir.dt.int32
AF = mybir.ActivationFunctionType
ALU = mybir.AluOpType
AX = mybir.AxisListType

D = 1024
S = 4096
NT = S // 128
DEPTH = 2
NE = 32
DE = 256
EPS = 1e-6
NCOL = 4112
W_IN_MAP = [
    (0, 384, 0, False),
    (384, 768, 384, False),
    (768, 1152, 768, False),
    (2304, 2688, 1152, False),
    (3328, 3584, 1536, False),
    (1152, 1536, 1792, False),
    (2688, 3072, 2176, False),
    (3584, 3840, 2560, False),
    (1536, 1920, 2816, False),
    (1920, 2304, 3200, False),
    (3072, 3200, 3584, True),
    (3200, 3328, 3840, True),
    (3840, 3856, 4096, False),
]
C_V = 768
C_G = 1792
C_HQ = 2816
C_HF = 3200
C_GQ = 3584
C_GK = 3840
C_GA = 4096


class Buf:
    __slots__ = ("name", "last_w", "readers")

    def __init__(self, name=""):
        self.name = name
        self.last_w = None
        self.readers = {}


class _Rec:
    def __init__(self):
        self.call = None

    def __getattr__(self, name):
        def f(*a, **kw):
            self.call = (name, a, kw)
            return self
        return f


def _free_elems(ap):
    n = 1
    for d_ in ap.shape[1:]:
        n *= d_
    return n


class Prog:
    N_DMA_SEMS = 24

    def __init__(self):
        self.nc = bass.Bass("TRN2", target_bir_lowering=False)
        nc = self.nc
        self.es = contextlib.ExitStack()
        self.eng = {"pe": nc.tensor, "act": nc.scalar, "dve": nc.vector, "pool": nc.gpsimd, "sp": nc.sync}
        self.sem = {}
        self.cnt = {}
        for e in self.eng:
            self.sem[e] = self.es.enter_context(nc.semaphore("s_" + e))
            self.cnt[e] = 0
        self.dma_sems = {}
        self.dma_rr = {}
        for q, n in (("sp", 16), ("pool", 16), ("act", 4)):
            self.dma_sems[q] = []
            self.dma_rr[q] = 0
            for j in range(n):
                k = ("dma", q, j)
                self.sem[k] = self.es.enter_context(nc.semaphore("s_dma_%s%d" % (q, j)))
                self.cnt[k] = 0
                self.dma_sems[q].append(k)
        self.waited = {e: {} for e in self.eng}
        self.n_wait = 0
        self.n_ins = 0
        self.stacks = []
        self.recording = False
        self.pending = []
        self.psum = self.es.enter_context(nc.psum_tensor("psum_all", [128, 4096], F32))
        self.bank_bufs = [Buf("bank%d" % i) for i in range(8)]
        self.bank_free = list(range(8))

    def sb(self, name, shape, dt, es=None):
        self.n_sb = getattr(self, "n_sb", 0) + 1
        return (es or self.es).enter_context(self.nc.sbuf_tensor("sb%d_%s" % (self.n_sb, name), list(shape), dt))

    def balloc(self):
        assert self.bank_free, "out of PSUM banks"
        i = self.bank_free.pop(0)
        return i

    def bfree(self, i):
        self.bank_free.append(i)

    def bank(self, i):
        return self.psum[:, i * 512:(i + 1) * 512]

    def bank_bf(self, i):
        return self.psum[:, i * 512:(i + 1) * 512].bitcast(BF16)

    def _wait(self, e, sk, v):
        if v <= 0:
            return
        if self.waited[e].get(sk, 0) >= v:
            return
        if sk == e and (e == "pe" or e == "sp"):
            return
        self.eng[e].wait_ge(self.sem[sk], v)
        self.waited[e][sk] = v
        self.n_wait += 1

    def _deps(self, e, reads, writes):
        for b in reads:
            if b.last_w is not None:
                self._wait(e, *b.last_w)
        for b in writes:
            if b.last_w is not None:
                self._wait(e, *b.last_w)
            for sk, v in b.readers.items():
                self._wait(e, sk, v)

    def _mark(self, tok, reads, writes):
        sk, v = tok
        for b in reads:
            if b.readers.get(sk, 0) < v:
                b.readers[sk] = v
        for b in writes:
            b.last_w = tok
            b.readers = {}

    def _cost(self, it):
        if it["kind"] == "dma":
            return 0.15, 2.5
        name, a, kw = it["call"]
        e = it["e"]
        out = a[0] if a else kw.get("out")
        try:
            n = _free_elems(out)
        except Exception:
            n = 256
        if e == "pe":
            c = 0.03 + n * 0.00045
        elif e == "act":
            c = 0.22 + n * 0.00105
        elif e == "dve":
            c = 0.13 + n * (0.0021 if name == "tensor_tensor_scan" else 0.00105)
        else:
            c = 0.35 + n * 0.0021
        return c, c

    def flush(self):
        items, self.pending = self.pending, []
        self.recording = False
        n = len(items)
        if n == 0:
            return
        lastw, readers = {}, {}
        last_pe = [None]
        preds = [set() for _ in range(n)]
        for i, it in enumerate(items):
            for b in it["reads"]:
                k = id(b)
                if k in lastw:
                    preds[i].add(lastw[k])
            for b in it["writes"]:
                k = id(b)
                if k in lastw:
                    preds[i].add(lastw[k])
                for r in readers.get(k, ()):
                    preds[i].add(r)
            for b in it["reads"]:
                readers.setdefault(id(b), []).append(i)
            for b in it["writes"]:
                lastw[id(b)] = i
                readers[id(b)] = []
            preds[i].discard(i)
            if KEEP_PE_ORDER and it["e"] == "pe":
                if last_pe[0] is not None:
                    preds[i].add(last_pe[0])
                last_pe[0] = i
        succs = [[] for _ in range(n)]
        indeg = [0] * n
        for i in range(n):
            indeg[i] = len(preds[i])
            for p in preds[i]:
                succs[p].append(i)
        ready = [i for i in range(n) if indeg[i] == 0]
        eng_free = {}
        finish = [0.0] * n
        est = [0.0] * n
        order = []
        LAT = 0.3
        WIN = 800
        lo = 0
        done = [False] * n
        while ready:
            while lo < n and done[lo]:
                lo += 1
            best, bkey = None, None
            for i in ready:
                if i > lo + WIN:
                    continue
                q = items[i]["e"]
                stt = max(eng_free.get(q, 0.0), est[i])
                key = (stt, i)
                if bkey is None or key < bkey:
                    best, bkey = i, key
            if best is None:
                best = min(ready)
                bkey = (max(eng_free.get(items[best]["e"], 0.0), est[best]), best)
            ready.remove(best)
            busy, lat = self._cost(items[best])
            q = items[best]["e"]
            eng_free[q] = bkey[0] + busy
            finish[best] = bkey[0] + lat
            done[best] = True
            order.append(best)
            for s_ in succs[best]:
                if items[s_]["e"] == q:
                    lat_ = 0.0 if q == "pe" else 0.1
                else:
                    lat_ = LAT
                est[s_] = max(est[s_], finish[best] + lat_)
                indeg[s_] -= 1
                if indeg[s_] == 0:
                    ready.append(s_)
        assert len(order) == n
        for i in order:
            it = items[i]
            if it["kind"] == "dma":
                self.dma(it["e"], it["out"], it["in_"], it["reads"], it["writes"], **it["kw"])
            else:
                name, a, kw = it["call"]
                self.op(it["e"], lambda eng, name=name, a=a, kw=kw: getattr(eng, name)(*a, **kw), it["reads"], it["writes"])

    def op(self, e, fn, reads=(), writes=()):
        if self.recording:
            r = _Rec()
            fn(r)
            self.pending.append(dict(kind="op", e=e, call=r.call, reads=list(reads), writes=list(writes)))
            return None
        self._deps(e, reads, writes)
        ins = fn(self.eng[e])
        self.cnt[e] += 1
        ins.then_inc(self.sem[e], 1)
        self._mark((e, self.cnt[e]), reads, writes)
        self.n_ins += 1
        return ins

    def dma(self, q, out, in_, reads=(), writes=(), **kw):
        if self.recording:
            self.pending.append(dict(kind="dma", e=q, out=out, in_=in_, kw=kw, reads=list(reads), writes=list(writes)))
            return None
        k = self.dma_sems[q][self.dma_rr[q]]
        self.dma_rr[q] = (self.dma_rr[q] + 1) % len(self.dma_sems[q])
        self._wait(q, k, self.cnt[k])
        self._deps(q, reads, writes)
        ins = self.eng[q].dma_start(out=out, in_=in_, **kw)
        self.cnt[k] += 16
        ins.then_inc(self.sem[k], 16)
        self._mark((k, self.cnt[k]), reads, writes)
        self.n_ins += 1
        return ins

    def barrier(self):
        if self.recording or self.pending:
            self.flush()
        for e in self.eng:
            for sk in self.sem:
                if sk != e:
                    self._wait(e, sk, self.cnt[sk])

    def finish(self, e="sp"):
        if self.recording or self.pending:
            self.flush()
        for sk in self.sem:
            if sk != e:
                self._wait(e, sk, self.cnt[sk])

    def close(self):
        self.es.close()


def _host_consts():
    inv_freq = (10000.0 ** (-np.arange(0, 64, 2, dtype=np.float32) / np.float32(64))).astype(np.float32)
    invf = np.broadcast_to(inv_freq[None, :], (128, 32)).astype(np.float32).copy()
    h = np.arange(6, dtype=np.float64)
    lg = np.log1p(-np.exp2(-5.0 - h))
    pos = (np.arange(128) % 64).astype(np.float64)
    xq = np.exp(lg[None, :] * (pos[:, None] - 31.0))
    xk = np.exp(lg[None, :] * (31.0 - pos[:, None])) * (64.0 ** -0.5)
    XQK = np.concatenate([np.repeat(xq, 64, axis=1), np.repeat(xk, 64, axis=1)], axis=1).astype(np.float32)
    cret = np.zeros((128, 3), np.float64)
    for p in range(3):
        for half in range(2):
            cret[half * 64:(half + 1) * 64, p] = np.exp(lg[2 * p + half] * 64.0)
    CRET2 = np.repeat(cret[:, :, None], 2, axis=2).astype(np.float32)
    return {"invf": invf, "xqk": XQK, "cret": CRET2.reshape(128, 6)}


class _Stop(Exception):
    pass


SCHED = True
KEEP_PE_ORDER = False


def build(n_layers=DEPTH, do_moe=True, dbg=False, n_tiles=NT, n_exp=NE, cut=0):
    P = Prog()

    def ck(k):
        if cut == k:
            raise _Stop()

    nc = P.nc
    op = P.op

    def din(name, shape, dt=F32):
        return nc.dram_tensor(name, list(shape), dt, kind="ExternalInput").ap()

    x_d = din("x", [S, D])
    c_d = din("c", [128, 8])
    pos_d = din("pos", [128, NT], I32)
    w_ada_d = din("w_ada", [DEPTH, D, 6 * D])
    b_ada_d = din("b_ada", [DEPTH, 6 * D])
    norm_mix_d = din("norm_mix", [DEPTH, D])
    norm_ffn_d = din("norm_ffn", [DEPTH, D])
    w_in_d = din("w_in", [DEPTH, D, 3856])
    ret_norm_d = din("ret_norm", [DEPTH, 384])
    hgrn_norm_d = din("hgrn_norm", [DEPTH, 384])
    lbl_d = din("hgrn_lb_logits", [DEPTH, 128, 3])
    wa2_d = din("gla_wa2", [DEPTH, 16, 256])
    gba_d = din("gla_ba", [DEPTH, 128, 2])
    gla_norm_d = din("gla_norm", [DEPTH, 256])
    w_out_d = din("w_out", [DEPTH, D, D])
    rgw_d = din("router_group_w", [DEPTH, D, 4])
    rgb_d = din("router_group_b", [DEPTH, 4])
    rew_d = din("router_expert_w", [DEPTH, D, 32])
    reb_d = din("router_expert_b", [DEPTH, 32])
    ewg_d = din("expert_w_gate", [DEPTH, NE, D, DE])
    ewu_d = din("expert_w_up", [DEPTH, NE, D, DE])
    ewd_d = din("expert_w_down", [DEPTH, NE, DE, D])
    nfin_d = din("norm_final", [D])
    invf_d = din("invf", [128, 32])
    xqk_d = din("xqk", [128, 768])
    cret_d = din("cret", [128, 6])
    out_d = nc.dram_tensor("out", [S, D], F32, kind="ExternalOutput").ap()
    skind = "ExternalOutput" if dbg else "Internal"
    xs_d = nc.dram_tensor("xs", [S, D], F32, kind=skind).ap()
    h2T_d = nc.dram_tensor("h2T", [128, 8, S], BF16, kind=skind).ap()
    comb_d = nc.dram_tensor("combd", [128, NT, 32], F32, kind=skind).ap() if dbg else None

    xs_bufs = [Buf("xs%d" % t) for t in range(NT)]
    h2T_bufs = [Buf("h2T%d" % t) for t in range(NT)]
    out_bufs = [Buf("out%d" % t) for t in range(NT)]

    sb = P.sb
    ident = sb("ident", [128, 128], BF16); b_ident = Buf("ident")
    mask4 = sb("mask4", [128, 4, 128], I32); b_mask = Buf("mask4")
    ones = sb("ones", [128, 128], F32); b_ones = Buf("ones")
    zeros = sb("zeros", [128, 8], F32); b_zeros = Buf("zeros")
    epsc = sb("epsc", [128, 1], F32); b_epsc = Buf("epsc")
    COS = sb("COS", [128, NT, 32], BF16); b_cos = Buf("COS")
    SIN = sb("SIN", [128, NT, 32], BF16); b_sin = Buf("SIN")
    XQK = sb("XQK", [128, 768], F32); b_xqk = Buf("XQK")
    cact = sb("cact", [128, 8], F32); b_cact = Buf("cact")
    mod = sb("mod", [128, 6, D], BF16); b_mod = [Buf("mod%d" % i) for i in range(6)]
    comb = sb("comb", [128, NT, 32], F32); b_comb = [Buf("comb%d" % t) for t in range(NT)]
    gain = sb("gain", [128, D], BF16); b_gain = Buf("gain")
    rbias = sb("rbias", [128, 36], F32); b_rbias = Buf("rbias")
    Wr = sb("Wr", [128, 8, 36], BF16); b_Wr = Buf("Wr")
    lbT = sb("lbT", [128, 2, 3], F32); b_lb = Buf("lbT")
    wa2p = sb("wa2p", [16, 256], BF16); b_wa2 = Buf("wa2p")
    nba = sb("nba", [128, 2], F32); b_nba = Buf("nba")
    cvec = [sb("cvec%d" % i, [128, 8, 2], F32) for i in range(2)]; b_cvec = [Buf("cvec0"), Buf("cvec1")]

    op("pool", lambda e: e.memset(ident[:], 0.0), writes=[b_ident])
    op("pool", lambda e: e.affine_select(ident[:], ident[:], [[-1, 128]], ALU.not_equal, 1.0, base=0, channel_multiplier=1),
       reads=[b_ident], writes=[b_ident])
    op("pool", lambda e: e.memset(mask4[:], 1), writes=[b_mask])
    for g in range(4):
        op("pool", lambda e: e.affine_select(mask4[:, g, :], mask4[:, g, :], [[1, 128]], ALU.is_ge, 0.0, base=0, channel_multiplier=-1),
           reads=[b_mask], writes=[b_mask])
        op("pool", lambda e: e.memset(mask4[0:64, g, 64:128], 0), reads=[b_mask], writes=[b_mask])
    if dbg:
        op("pool", lambda e: e.memset(comb[:], 0.0), writes=b_comb)
    op("pool", lambda e: e.memset(ones[:], 1.0), writes=[b_ones])
    op("pool", lambda e: e.memset(zeros[:], 0.0), writes=[b_zeros])
    op("pool", lambda e: e.memset(epsc[:], EPS), writes=[b_epsc])
    P.dma("sp", XQK[:], xqk_d, writes=[b_xqk])
    for i in range(2):
        P.dma("sp", cvec[i][:, 0:3, :], cret_d.rearrange("p (a b) -> p a b", a=3), writes=[b_cvec[i]])

    def setup_tables(es0):
        c_sb = sb("c_sb", [128, 8], F32, es0); b_c = Buf("c")
        P.dma("sp", c_sb[:], c_d, writes=[b_c])
        op("act", lambda e: e.activation(c_sb[:], c_sb[:], AF.Silu), reads=[b_c], writes=[b_c])
        op("dve", lambda e: e.tensor_copy(cact[:], c_sb[:]), reads=[b_c], writes=[b_cact])
        pos_i = sb("pos_i", [128, NT], I32, es0); b_pi = Buf("pos_i")
        pos_f = sb("pos_f", [128, NT], F32, es0); b_pf = Buf("pos_f")
        invf = sb("invf", [128, 32], F32, es0); b_if = Buf("invf")
        ang = sb("ang", [128, NT, 32], F32, es0); b_ang = Buf("ang")
        kk_i = sb("kk_i", [128, NT, 32], I32, es0); b_kki = Buf("kki")
        kk = sb("kk", [128, NT, 32], F32, es0); b_kk = Buf("kk")
        rr = sb("rr", [128, NT, 32], F32, es0); b_rr = Buf("rr")
        mm = sb("mm", [128, NT, 32], F32, es0); b_mm = Buf("mm")
        P.dma("sp", pos_i[:], pos_d, writes=[b_pi])
        P.dma("sp", invf[:], invf_d, writes=[b_if])
        op("dve", lambda e: e.tensor_copy(pos_f[:], pos_i[:]), reads=[b_pi], writes=[b_pf])
        op("dve", lambda e: e.tensor_tensor(ang[:], pos_f[:].unsqueeze(2).broadcast_to([128, NT, 32]),
                                            invf[:].unsqueeze(1).broadcast_to([128, NT, 32]), ALU.mult),
           reads=[b_pf, b_if], writes=[b_ang])
        TWO_PI = 2.0 * math.pi
        HI = 6.28125
        LO = TWO_PI - HI

        def wrap(dst_tab, shift):
            op("dve", lambda e: e.tensor_scalar(rr[:], ang[:], shift, 1.0 / TWO_PI, ALU.add, ALU.mult), reads=[b_ang], writes=[b_rr])
            op("dve", lambda e: e.tensor_copy(kk_i[:], rr[:]), reads=[b_rr], writes=[b_kki])
            op("dve", lambda e: e.tensor_copy(kk[:], kk_i[:]), reads=[b_kki], writes=[b_kk])
            op("dve", lambda e: e.scalar_tensor_tensor(rr[:], kk[:], -HI, ang[:], ALU.mult, ALU.add), reads=[b_kk, b_ang], writes=[b_rr])
            op("dve", lambda e: e.scalar_tensor_tensor(rr[:], kk[:], -LO, rr[:], ALU.mult, ALU.add), reads=[b_kk, b_rr], writes=[b_rr])
            if shift != 0.0:
                op("dve", lambda e: e.tensor_scalar(rr[:], rr[:], shift, None, ALU.add), reads=[b_rr], writes=[b_rr])
            op("dve", lambda e: e.tensor_single_scalar(mm[:], rr[:], math.pi, ALU.is_gt), reads=[b_rr], writes=[b_mm])
            op("dve", lambda e: e.scalar_tensor_tensor(rr[:], mm[:], -TWO_PI, rr[:], ALU.mult, ALU.add), reads=[b_mm, b_rr], writes=[b_rr])
            op("dve", lambda e: e.tensor_single_scalar(mm[:], rr[:], -math.pi, ALU.is_lt), reads=[b_rr], writes=[b_mm])
            op("dve", lambda e: e.scalar_tensor_tensor(rr[:], mm[:], TWO_PI, rr[:], ALU.mult, ALU.add), reads=[b_mm, b_rr], writes=[b_rr])
            op("dve", lambda e: e.tensor_scalar(rr[:], rr[:], 3.1415925, -3.1415925, ALU.min, ALU.max), reads=[b_rr], writes=[b_rr])
            return op("act", lambda e: e.activation(dst_tab[0][:], rr[:], AF.Sin), reads=[b_rr], writes=[dst_tab[1]])

        wrap((SIN, b_sin), 0.0)
        wrap((COS, b_cos), math.pi / 2.0)

    def layer(l, last):
        esA = contextlib.ExitStack(); P.stacks.append(esA)
        w_in = sb("w_in", [128, 8, NCOL], BF16, esA); b_win = Buf("w_in")
        w_out = sb("w_out", [128, 8, D], BF16, esA); b_wout = Buf("w_out")

        op("pool", lambda e: e.memset(w_in[:, :, C_GQ:C_GA], 0.0), writes=[b_win])
        for (slo, shi, dlo, pad) in W_IN_MAP:
            src = w_in_d[l, :, slo:shi].rearrange("(k p) f -> p k f", p=128)
            if not pad:
                P.dma("pool", w_in[:, :, dlo:dlo + (shi - slo)], src, reads=[], writes=[b_win])
            else:
                for h in range(4):
                    P.dma("pool", w_in[:, :, dlo + h * 64:dlo + h * 64 + 32],
                          w_in_d[l, :, slo + h * 32:slo + (h + 1) * 32].rearrange("(k p) f -> p k f", p=128), writes=[b_win])
        P.dma("pool", w_out[:], w_out_d[l].rearrange("(k p) f -> p k f", p=128), writes=[b_wout])
        P.dma("pool", Wr[:, :, 0:4], rgw_d[l].rearrange("(k p) f -> p k f", p=128), writes=[b_Wr])
        P.dma("pool", Wr[:, :, 4:36], rew_d[l].rearrange("(k p) f -> p k f", p=128), writes=[b_Wr])
        P.dma("sp", rbias[:, 0:4], rgb_d[l].partition_broadcast(128), writes=[b_rbias])
        P.dma("sp", rbias[:, 4:36], reb_d[l].partition_broadcast(128), writes=[b_rbias])
        P.dma("pool", gain[:, 0:384], ret_norm_d[l].partition_broadcast(128), writes=[b_gain])
        P.dma("pool", gain[:, 384:768], hgrn_norm_d[l].partition_broadcast(128), writes=[b_gain])
        P.dma("pool", gain[:, 768:1024], gla_norm_d[l].partition_broadcast(128), writes=[b_gain])
        if l == 0:
            op("pool", lambda e: e.memset(lbT[:, 0, :], 0.0), writes=[b_lb])
            op("pool", lambda e: e.memset(lbT[:, 1, :], 0.0), reads=[b_lb], writes=[b_lb])
        else:
            P.dma("sp", lbT[:, 0, :], lbl_d[0], writes=[b_lb])
            P.dma("sp", lbT[:, 1, :], lbl_d[1], writes=[b_lb])
            op("dve", lambda e: e.tensor_tensor(lbT[:, 0, :], lbT[:, 1, :], lbT[:, 0, :], ALU.subtract), reads=[b_lb], writes=[b_lb])
            op("act", lambda e: e.activation(lbT[:, 0, :], lbT[:, 0, :], AF.Sigmoid), reads=[b_lb], writes=[b_lb])
            op("dve", lambda e: e.tensor_scalar(lbT[:, 1, :], lbT[:, 0, :], -1.0, 1.0, ALU.mult, ALU.add), reads=[b_lb], writes=[b_lb])
            op("act", lambda e: e.activation(lbT[:, 1, :], lbT[:, 1, :], AF.Ln), reads=[b_lb], writes=[b_lb])
        P.dma("pool", wa2p[:], wa2_d[l], writes=[b_wa2])
        P.dma("sp", nba[:], gba_d[l], writes=[b_nba])
        op("dve", lambda e: e.tensor_scalar(nba[:], nba[:], -1.0, None, ALU.mult), reads=[b_nba], writes=[b_nba])

        with contextlib.ExitStack() as es1:
            P.stacks.append(es1)
            if l == 0:
                setup_tables(es1)
            wad = [sb("wad%d" % i, [128, 8, 256], BF16, es1) for i in range(2)]
            b_wad = [Buf("wad0"), Buf("wad1")]
            wst = [sb("wst%d" % i, [128, 8, 256], F32, es1) for i in range(2)]
            b_wst = [Buf("wst0"), Buf("wst1")]
            bad = sb("bad", [128, 6 * D], F32, es1); b_bad = Buf("bad")
            gmx = sb("gmx", [128, 2, D], F32, es1); b_gmx = Buf("gmx")
            P.dma("sp", bad[:], b_ada_d[l].partition_broadcast(128), writes=[b_bad])
            P.dma("sp", gmx[:, 0, :], norm_mix_d[l].partition_broadcast(128), writes=[b_gmx])
            P.dma("sp", gmx[:, 1, :], norm_ffn_d[l].partition_broadcast(128), writes=[b_gmx])
            crep = sb("crep", [128, 8, 128], BF16, es1); b_crep = Buf("crep")
            op("dve", lambda e: e.tensor_copy(crep[:], cact[:].unsqueeze(2).broadcast_to([128, 8, 128])), reads=[b_cact], writes=[b_crep])
            for cb in range(24):
                w = wad[cb % 2]; bw = b_wad[cb % 2]
                ws_, bws_ = wst[cb % 2], b_wst[cb % 2]
                P.dma("sp", ws_[:], w_ada_d[l, :, cb * 256:(cb + 1) * 256].rearrange("(k p) f -> p k f", p=128), writes=[bws_])
                if cb % 2 == 0:
                    op("act", lambda e: e.copy(w[:], ws_[:]), reads=[bws_], writes=[bw])
                else:
                    op("dve", lambda e: e.tensor_copy(w[:], ws_[:]), reads=[bws_], writes=[bw])
                bk = P.balloc()
                for k in range(8):
                    op("pe", lambda e: e.matmul(P.bank(bk)[:, 0:256], crep[:, k, :], w[:, k, :], start=(k == 0), stop=(k == 7)),
                       reads=[b_crep, bw], writes=[P.bank_bufs[bk]])
                op("dve", lambda e: e.tensor_tensor(bad[:, cb * 256:(cb + 1) * 256], P.bank(bk)[:, 0:256], bad[:, cb * 256:(cb + 1) * 256], ALU.add),
                   reads=[P.bank_bufs[bk], b_bad], writes=[b_bad])
                P.bfree(bk)
            op("dve", lambda e: e.scalar_tensor_tensor(bad[:, D:2 * D], bad[:, D:2 * D], 1.0, gmx[:, 0, :], ALU.add, ALU.mult),
               reads=[b_bad, b_gmx], writes=[b_bad])
            op("dve", lambda e: e.scalar_tensor_tensor(bad[:, 4 * D:5 * D], bad[:, 4 * D:5 * D], 1.0, gmx[:, 1, :], ALU.add, ALU.mult),
               reads=[b_bad, b_gmx], writes=[b_bad])
            op("act", lambda e: e.copy(mod[:, 0:3, :].rearrange("p a f -> p (a f)"), bad[:, 0:3 * D]), reads=[b_bad], writes=b_mod[0:3])
            op("dve", lambda e: e.tensor_copy(mod[:, 3:6, :].rearrange("p a f -> p (a f)"), bad[:, 3 * D:6 * D]), reads=[b_bad], writes=b_mod[3:6])
            P.barrier()
        shift_m, A_m, gate_m, shift_f, A_f, gate_f = [mod[:, i, :] for i in range(6)]
        ck(2)

        def two(name, shape, dt):
            return [sb("%s%d" % (name, i), shape, dt, esA) for i in range(2)], [Buf("%s%d" % (name, i)) for i in range(2)]
        xt, b_xt = two("xt", [128, D], F32)
        st, b_st = two("st", [128, 4], F32)
        hn = sb("hn", [128, D], F32, esA); b_hn = Buf("hn")
        h_bf = sb("h_bf", [128, D], BF16, esA); b_hbf = Buf("h_bf")
        hnA = sb("hnA", [128, D], F32, esA); b_hnA = Buf("hnA")
        h_bfA = sb("h_bfA", [128, D], BF16, esA); b_hbfA = Buf("h_bfA")
        def one(name, shape, dt):
            t_ = sb(name, shape, dt, esA); b_ = Buf(name)
            return [t_, t_], [b_, b_]
        hT, b_hT = one("hT", [128, 8, 128], BF16)
        qk_s = sb("qk_s", [128, 768], F32, esA); b_qks = Buf("qk_s")
        rt = [sb("rt%d" % i, [128, 12, 32], F32, esA) for i in range(4)]; b_rt = [Buf("rt%d" % i) for i in range(4)]
        QK, b_QK = two("QK", [128, 384 + 1024], BF16)
        QKT, b_QKT = two("QKT", [128, 2, 8, 128], BF16)
        Vall, b_V = two("Vall", [128, D], BF16)
        GG, b_GG = two("GG", [128, D], BF16)
        ff = sb("ff", [128, 3, 128], F32, esA); b_ff = Buf("ff")
        L1 = sb("L1", [128, 3, 128], F32, esA); b_L1 = Buf("L1")
        silq = sb("silq", [128, 3, 128], F32, esA); b_silq = Buf("silq")
        lf = sb("lf", [128, 5, 128], F32, esA); b_lf = Buf("lf")
        Gc, b_G = two("Gc", [128, 5, 128], F32)
        Ee, b_E = lf, b_lf
        qfac = sb("qfac", [128, 5, 128], F32, esA); b_qf = Buf("qfac")
        kfac = sb("kfac", [128, 5, 128], F32, esA); b_kf = Buf("kfac")
        dd = sb("dd", [128, 5, 2], F32, esA); b_dd = Buf("dd")
        gaT = sb("gaT", [16, 128], BF16, esA); b_gaT = Buf("gaT")
        exg = sb("exg", [128, 2, 128], F32, esA); b_exg = Buf("exg")
        PT, b_PT = one("PT", [128, 16, 128], BF16)
        Sbf = sb("Sbf", [128, 2, 8, 128], BF16, esA); b_Sbf = [Buf("Sbf0"), Buf("Sbf1")]
        Wst = sb("Wst", [128, 8, 128], F32, esA); b_W = Buf("W")
        tmpW, b_tW = hn[:].rearrange("p (a d) -> p a d", a=8), b_hn
        o_sb = sb("o_sb", [128, D], F32, esA); b_osb = Buf("o_sb")
        sq, b_sq = hn, b_hn
        hs = sb("hs", [128, 2, 16], F32, esA); b_hs = Buf("hs")
        merged = sb("merged", [128, D], BF16, esA); b_mg = Buf("merged")
        junk, b_junk = merged, b_mg
        mT = sb("mT", [128, 8, 128], BF16, esA); b_mT = Buf("mT")
        x1, b_x1 = xt, b_xt
        h2T, b_h2T = one("h2Ts", [128, 8, 128], BF16)
        rl = sb("rl", [128, 36], F32, esA); b_rl = Buf("rl")
        rs = sb("rs", [128, 16], F32, esA); b_rs = Buf("rs")
        rm = sb("rm", [128, 3, 32], F32, esA); b_rm = Buf("rm")
        top8 = sb("top8", [128, 8], F32, esA); b_top8 = Buf("top8")

        op("pool", lambda e: e.memset(Wst[:], 0.0), writes=[b_W])
        op("pool", lambda e: e.memset(PT[0][:], 0.0), writes=[b_PT[0]])

        x_src = x_d if l == 0 else xs_d
        hnB, b_hnB, h_bfB, b_hbfB, junkB_, b_junkB_ = hn, b_hn, h_bf, b_hbf, junk, b_junk

        def rmsnorm_to_bf(xin, b_xin, stt, b_stt, col, A, b_A, shift, b_shift, first):
            if first:
                hn, b_hn, h_bf, b_hbf, junk, b_junk = hnA, b_hnA, h_bfA, b_hbfA, h_bfA, b_hbfA
            else:
                hn, b_hn, h_bf, b_hbf, junk, b_junk = hnB, b_hnB, h_bfB, b_hbfB, junkB_, b_junkB_
            op("act", lambda e: e.activation(junk[:], xin[:], AF.Square, accum_out=stt[:, col:col + 1]),
               reads=[b_xin], writes=[b_junk, b_stt])
            op("act", lambda e: e.activation(stt[:, col + 1:col + 2], stt[:, col:col + 1], AF.Ln, bias=epsc[:, 0:1], scale=1.0 / D),
               reads=[b_stt], writes=[b_stt])
            op("act", lambda e: e.activation(stt[:, col + 1:col + 2], stt[:, col + 1:col + 2], AF.Exp, scale=-0.5), reads=[b_stt], writes=[b_stt])
            op("dve", lambda e: e.scalar_tensor_tensor(hn[:], xin[:], stt[:, col + 1:col + 2], A, ALU.mult, ALU.mult),
               reads=[b_xin, b_stt, b_A], writes=[b_hn])
            op("dve", lambda e: e.tensor_tensor(h_bf[:], hn[:], shift, ALU.add), reads=[b_hn, b_shift], writes=[b_hbf])

        def transpose8(src, b_src, dst, b_dst):
            bk = P.balloc()
            for k in range(8):
                op("pe", lambda e: e.transpose(P.bank_bf(bk)[:, k * 128:(k + 1) * 128], src[:, k * 128:(k + 1) * 128], ident[:]),
                   reads=[b_src, b_ident], writes=[P.bank_bufs[bk]])
            op("act", lambda e: e.copy(dst[:].rearrange("p k t -> p (k t)"), P.bank_bf(bk)), reads=[P.bank_bufs[bk]], writes=[b_dst])
            P.bfree(bk)

        def mm_tm(bk, lhs, b_lhs, c0, n):
            for k in range(8):
                op("pe", lambda e: e.matmul(P.bank(bk)[:, 0:n], lhs[:, k, :], w_in[:, k, c0:c0 + n], start=(k == 0), stop=(k == 7)),
                   reads=[b_lhs, b_win], writes=[P.bank_bufs[bk]])

        def mm_fm(bk, col, lhs, b_lhs, c0, m=128):
            for k in range(8):
                op("pe", lambda e: e.matmul(P.bank(bk)[0:m, col:col + 128], w_in[:, k, c0:c0 + m], lhs[:, k, :], start=(k == 0), stop=(k == 7)),
                   reads=[b_lhs, b_win], writes=[P.bank_bufs[bk]])

        def make_stages(t):
            b = t % 2
            pb = 1 - b
            BB = P.bank_bufs
            kTok = QK[b][:, 384:384 + 1024].rearrange("p (a d) -> p a d", a=8)
            def s12():
                P.dma("sp", xt[b][:], x_src[t * 128:(t + 1) * 128, :], reads=[xs_bufs[t]] if l > 0 else [], writes=[b_xt[b]])
                rmsnorm_to_bf(xt[b], b_xt[b], st[b], b_st[b], 0, A_m, b_mod[1], shift_m, b_mod[0], True)
                transpose8(h_bfA, b_hbfA, hT[b], b_hT[b])
                return None
            def s3():
                bq = P.balloc(); mm_tm(bq, hT[b], b_hT[b], 0, 384)
                bkk = P.balloc(); mm_tm(bkk, hT[b], b_hT[b], 384, 384)
                op("dve", lambda e: e.tensor_tensor(qk_s[:, 0:384], P.bank(bq)[:, 0:384], XQK[:, 0:384], ALU.mult),
                   reads=[BB[bq], b_xqk], writes=[b_qks])
                op("dve", lambda e: e.tensor_tensor(qk_s[:, 384:768], P.bank(bkk)[:, 0:384], XQK[:, 384:768], ALU.mult),
                   reads=[BB[bkk], b_xqk], writes=[b_qks])
                P.bfree(bq); P.bfree(bkk)
                qv = qk_s[:].rearrange("p (h two f) -> p h two f", two=2, f=32)
                x1v, x2v = qv[:, :, 0, :], qv[:, :, 1, :]
                cosb = COS[:, t, :].unsqueeze(1).broadcast_to([128, 12, 32])
                sinb = SIN[:, t, :].unsqueeze(1).broadcast_to([128, 12, 32])
                ov = QK[b][:, 0:768].rearrange("p (h two f) -> p h two f", two=2, f=32)
                op("pool", lambda e: e.tensor_tensor(rt[0][:], x1v, cosb, ALU.mult), reads=[b_qks, b_cos], writes=[b_rt[0]])
                op("pool", lambda e: e.tensor_tensor(rt[1][:], x2v, sinb, ALU.mult), reads=[b_qks, b_sin], writes=[b_rt[1]])
                op("dve", lambda e: e.tensor_tensor(ov[:, :, 0, :], rt[0][:], rt[1][:], ALU.subtract), reads=[b_rt[0], b_rt[1]], writes=[b_QK[b]])
                op("pool", lambda e: e.tensor_tensor(rt[2][:], x1v, sinb, ALU.mult), reads=[b_qks, b_sin], writes=[b_rt[2]])
                op("pool", lambda e: e.tensor_tensor(rt[3][:], x2v, cosb, ALU.mult), reads=[b_qks, b_cos], writes=[b_rt[3]])
                op("dve", lambda e: e.tensor_tensor(ov[:, :, 1, :], rt[2][:], rt[3][:], ALU.add), reads=[b_rt[2], b_rt[3]], writes=[b_QK[b]])
                bk = P.balloc()
                for j in range(6):
                    op("pe", lambda e: e.transpose(P.bank_bf(bk)[:, j * 128:(j + 1) * 128], QK[b][:, j * 128:(j + 1) * 128], ident[:]),
                       reads=[b_QK[b], b_ident], writes=[BB[bk]])
                op("act", lambda e: e.copy(QKT[b][:, :, 0:3, :].rearrange("p a c t -> p a (c t)"),
                                           P.bank_bf(bk)[:, 0:768].rearrange("p (a x) -> p a x", a=2)),
                   reads=[BB[bk]], writes=[b_QKT[b]])
                P.bfree(bk)
                return None
            def s4a():
                bhq = P.balloc()
                for p in range(3):
                    mm_fm(bhq, p * 128, hT[b], b_hT[b], C_HQ + p * 128)
                bhf = P.balloc()
                for p in range(3):
                    mm_fm(bhf, p * 128, hT[b], b_hT[b], C_HF + p * 128)
                bgl = P.balloc()
                for p in range(2):
                    mm_fm(bgl, p * 128, hT[b], b_hT[b], C_GQ + p * 128)
                for p in range(2):
                    mm_fm(bgl, 256 + p * 128, hT[b], b_hT[b], C_GK + p * 128)
                bga = P.balloc()
                mm_fm(bga, 0, hT[b], b_hT[b], C_GA, m=16)
                op("act", lambda e: e.activation(ff[:].rearrange("p a t -> p (a t)"), P.bank(bhf)[:, 0:384], AF.Exp, scale=-1.0),
                   reads=[BB[bhf]], writes=[b_ff])
                op("act", lambda e: e.activation(L1[:], ff[:], AF.Ln, bias=1.0), reads=[b_ff], writes=[b_L1])
                if l == 0:
                    op("dve", lambda e: e.tensor_scalar(lf[:, 0:3, :], L1[:], -1.0, -80.0, ALU.mult, ALU.max), reads=[b_L1], writes=[b_lf])
                else:
                    for p in range(3):
                        op("act", lambda e: e.activation(ff[:, p, :], ff[:, p, :], AF.Ln, bias=1.0, scale=lbT[:, 0, p:p + 1]),
                           reads=[b_ff, b_lb], writes=[b_ff])
                    op("dve", lambda e: e.tensor_tensor(lf[:, 0:3, :], ff[:], L1[:], ALU.subtract), reads=[b_ff, b_L1], writes=[b_lf])
                    op("dve", lambda e: e.tensor_scalar(lf[:, 0:3, :], lf[:, 0:3, :], -80.0, None, ALU.max), reads=[b_lf], writes=[b_lf])
                op("dve", lambda e: e.tensor_tensor(L1[:].rearrange("p a t -> p (a t)"), P.bank(bhf)[:, 0:384],
                                                    L1[:].rearrange("p a t -> p (a t)"), ALU.add),
                   reads=[BB[bhf], b_L1, b_lf], writes=[b_L1])
                P.bfree(bhf)
                op("act", lambda e: e.activation(silq[:].rearrange("p a t -> p (a t)"), P.bank(bhq)[:, 0:384], AF.Exp, scale=-1.0),
                   reads=[BB[bhq]], writes=[b_silq])
                op("act", lambda e: e.activation(silq[:], silq[:], AF.Ln, bias=1.0), reads=[b_silq], writes=[b_silq])
                op("act", lambda e: e.copy(gaT[:], P.bank(bga)[0:16, 0:128]), reads=[BB[bga]], writes=[b_gaT])
                for p in range(2):
                    op("pe", lambda e: e.matmul(P.bank(bga)[:, 128 + p * 128:256 + p * 128], wa2p[:, p * 128:(p + 1) * 128], gaT[:], start=True, stop=True),
                       reads=[b_wa2, b_gaT], writes=[BB[bga]])
                for p in range(2):
                    op("act", lambda e: e.activation(exg[:, p, :], P.bank(bga)[:, 128 + p * 128:256 + p * 128], AF.Exp, bias=nba[:, p:p + 1], scale=-1.0),
                       reads=[BB[bga], b_nba], writes=[b_exg])
                P.bfree(bga)
                op("act", lambda e: e.activation(exg[:], exg[:], AF.Ln, bias=1.0), reads=[b_exg], writes=[b_exg])
                op("dve", lambda e: e.tensor_scalar(lf[:, 3:5, :], exg[:], -1.0 / 16.0, -80.0, ALU.mult, ALU.max), reads=[b_exg], writes=[b_lf])
                for p in range(5):
                    init = zeros[:, 0:1] if t == 0 else Gc[pb][:, p, 127:128]
                    rd = [b_ones, b_lf, b_zeros] if t == 0 else [b_ones, b_lf, b_G[pb]]
                    op("dve", lambda e: e.tensor_tensor_scan(Gc[b][:, p, :], ones[:], lf[:, p, :], init, ALU.mult, ALU.add),
                       reads=rd, writes=[b_G[b]])
                G4 = Gc[b][:].rearrange("p a (c s) -> p a c s", c=2)
                E4 = Ee[:].rearrange("p a (c s) -> p a c s", c=2)
                op("dve", lambda e: e.tensor_tensor(E4, G4, G4[:, :, :, 31:32].broadcast_to([128, 5, 2, 64]), ALU.subtract),
                   reads=[b_G[b]], writes=[b_E])
                op("dve", lambda e: e.tensor_scalar(Ee[:], Ee[:], 80.0, -80.0, ALU.min, ALU.max), reads=[b_E], writes=[b_E])
                op("dve", lambda e: e.tensor_tensor(silq[:], Ee[:, 0:3, :], silq[:], ALU.subtract), reads=[b_E, b_silq], writes=[b_silq])
                op("dve", lambda e: e.tensor_tensor(L1[:], L1[:], Ee[:, 0:3, :], ALU.add), reads=[b_E, b_L1], writes=[b_L1])
                op("act", lambda e: e.activation(qfac[:, 0:3, :], silq[:], AF.Exp), reads=[b_silq], writes=[b_qf])
                if l == 0:
                    op("act", lambda e: e.activation(QKT[b][:, 1, 3:6, :], L1[:], AF.Exp, scale=-1.0), reads=[b_L1], writes=[b_QKT[b]])
                else:
                    for p in range(3):
                        op("act", lambda e: e.activation(QKT[b][:, 1, 3 + p, :], L1[:, p, :], AF.Exp, scale=-1.0, bias=lbT[:, 1, p:p + 1]),
                           reads=[b_L1, b_lb], writes=[b_QKT[b]])
                op("act", lambda e: e.activation(qfac[:, 3:5, :], Ee[:, 3:5, :], AF.Exp), reads=[b_E], writes=[b_qf])
                op("act", lambda e: e.activation(kfac[:, 3:5, :], Ee[:, 3:5, :], AF.Exp, scale=-1.0), reads=[b_E], writes=[b_kf])
                prev_mid = zeros[:, 0:5] if t == 0 else Gc[pb][:, :, 95]
                rdp = [b_zeros] if t == 0 else [b_G[pb]]
                op("dve", lambda e: e.tensor_tensor(dd[:, :, 0], Gc[b][:, :, 31], prev_mid, ALU.subtract), reads=[b_G[b]] + rdp, writes=[b_dd])
                op("dve", lambda e: e.tensor_tensor(dd[:, :, 1], Gc[b][:, :, 95], Gc[b][:, :, 31], ALU.subtract), reads=[b_G[b], b_dd], writes=[b_dd])
                op("act", lambda e: e.activation(cvec[b][:, 3:8, :], dd[:], AF.Exp), reads=[b_dd], writes=[b_cvec[b]])
                op("dve", lambda e: e.scalar_tensor_tensor(QKT[b][:, 0, 3:6, :], P.bank(bhq)[:, 0:384].rearrange("p (a t) -> p a t", a=3),
                                                           0.125, qfac[:, 0:3, :], ALU.mult, ALU.mult),
                   reads=[BB[bhq], b_qf], writes=[b_QKT[b]])
                P.bfree(bhq)
                op("dve", lambda e: e.scalar_tensor_tensor(QKT[b][:, 0, 6:8, :], P.bank(bgl)[:, 0:256].rearrange("p (a t) -> p a t", a=2),
                                                           32.0 ** -0.5, qfac[:, 3:5, :], ALU.mult, ALU.mult),
                   reads=[BB[bgl], b_qf], writes=[b_QKT[b]])
                op("dve", lambda e: e.tensor_tensor(QKT[b][:, 1, 6:8, :], P.bank(bgl)[:, 256:512].rearrange("p (a t) -> p a t", a=2),
                                                    kfac[:, 3:5, :], ALU.mult),
                   reads=[BB[bgl], b_kf], writes=[b_QKT[b]])
                P.bfree(bgl)
                bk = P.balloc()
                for j in range(5):
                    op("pe", lambda e: e.transpose(P.bank_bf(bk)[:, j * 128:(j + 1) * 128], QKT[b][:, 1, 3 + j, :], ident[:]),
                       reads=[b_QKT[b], b_ident], writes=[BB[bk]])
                op("act", lambda e: e.copy(QK[b][:, 384 + 384:384 + 1024], P.bank_bf(bk)[:, 0:640]), reads=[BB[bk]], writes=[b_QK[b]])
                P.bfree(bk)
                return None
            def s4b():
                for hf in range(2):
                    bk = P.balloc(); mm_tm(bk, hT[b], b_hT[b], C_V + hf * 512, 512)
                    op("act", lambda e: e.copy(Vall[b][:, hf * 512:(hf + 1) * 512], P.bank(bk)), reads=[BB[bk]], writes=[b_V[b]])
                    P.bfree(bk)
                for hf in range(2):
                    bk = P.balloc(); mm_tm(bk, hT[b], b_hT[b], C_G + hf * 512, 512)
                    op("act", lambda e: e.activation(GG[b][:, hf * 512:(hf + 1) * 512], P.bank(bk), AF.Silu), reads=[BB[bk]], writes=[b_GG[b]])
                    P.bfree(bk)
                op("pool", lambda e: e.tensor_tensor(GG[b][:], GG[b][:], gain[:], ALU.mult), reads=[b_GG[b], b_gain], writes=[b_GG[b]])
                return None
            def s5():
                kTok = QK[b][:, 384:384 + 1024].rearrange("p (a d) -> p a d", a=8)
                for c in range(2):
                    bu = [P.balloc(), P.balloc()]
                    for p in range(8):
                        op("pe", lambda e: e.matmul(P.bank(bu[p // 4])[:, (p % 4) * 128:(p % 4 + 1) * 128],
                                                    kTok[64 * c:64 * c + 64, p, :], Vall[b][64 * c:64 * c + 64, p * 128:(p + 1) * 128],
                                                    start=True, stop=True),
                           reads=[b_QK[b], b_V[b]], writes=[BB[bu[p // 4]]])
                    op("dve", lambda e: e.tensor_tensor(tmpW, Wst[:], cvec[b][:, :, c:c + 1].broadcast_to([128, 8, 128]), ALU.mult),
                       reads=[b_W, b_cvec[b]], writes=[b_tW])
                    op("act", lambda e: e.copy(Sbf[:, c, :, :], tmpW), reads=[b_tW], writes=[b_Sbf[c]])
                    for g in range(2):
                        op("dve", lambda e: e.tensor_tensor(Wst[:, 4 * g:4 * g + 4, :].rearrange("p a d -> p (a d)"),
                                                            tmpW[:, 4 * g:4 * g + 4, :].rearrange("p a d -> p (a d)"), P.bank(bu[g]), ALU.add),
                           reads=[b_tW, BB[bu[g]]], writes=[b_W])
                    P.bfree(bu[0]); P.bfree(bu[1])
                return None
            def s6():
                for half in range(2):
                    for g in range(2):
                        bk = P.balloc()
                        for hh in range(4):
                            h = 8 * g + 2 * hh + half
                            p = h // 2
                            op("pe", lambda e: e.matmul(P.bank(bk)[:, hh * 128:(hh + 1) * 128],
                                                        QKT[b][64 * half:64 * half + 64, 1, p, :], QKT[b][64 * half:64 * half + 64, 0, p, :],
                                                        start=True, stop=True),
                               reads=[b_QKT[b]], writes=[BB[bk]])
                        i0 = half * 8 + g * 4
                        op("dve", lambda e: e.copy_predicated(PT[b][:, i0:i0 + 4, :].rearrange("p a t -> p (a t)"),
                                                              mask4[:].rearrange("p a t -> p (a t)"), P.bank(bk)),
                           reads=[BB[bk], b_mask, b_PT[b]], writes=[b_PT[b]])
                        P.bfree(bk)
                return None
            def s7():
                bo = [P.balloc(), P.balloc()]
                for half in range(2):
                    for j in range(8):
                        h = 2 * j + half
                        pi = (h % 2) * 8 + (h // 8) * 4 + (h % 8) // 2
                        op("pe", lambda e: e.matmul(P.bank(bo[half])[:, j * 64:j * 64 + 64], PT[b][:, pi, :], Vall[b][:, h * 64:(h + 1) * 64],
                                                    start=(j == 0), stop=False),
                           reads=[b_PT[b], b_V[b]], writes=[BB[bo[half]]])
                for half in range(2):
                    for j in range(8):
                        h = 2 * j + half
                        p = h // 2
                        for c in range(2):
                            op("pe", lambda e: e.matmul(P.bank(bo[half])[64 * c:64 * c + 64, j * 64:j * 64 + 64],
                                                        QKT[b][64 * half:64 * half + 64, 0, p, 64 * c:64 * c + 64],
                                                        Sbf[64 * half:64 * half + 64, c, p, 64 * half:64 * half + 64],
                                                        start=False, stop=(j == 7)),
                               reads=[b_QKT[b], b_Sbf[c]], writes=[BB[bo[half]]])
                ov4 = o_sb[:].rearrange("p (j two e) -> p j two e", two=2, e=64)
                for half in range(2):
                    op("act", lambda e: e.copy(ov4[:, :, half, :], P.bank(bo[half]).rearrange("p (j e) -> p j e", e=64)),
                       reads=[BB[bo[half]]], writes=[b_osb])
                    P.bfree(bo[half])
                op("act", lambda e: e.activation(sq[:], o_sb[:], AF.Square), reads=[b_osb], writes=[b_sq])
                op("dve", lambda e: e.tensor_reduce(hs[:, 0, :], sq[:].rearrange("p (h e) -> p h e", e=64), AX.X, ALU.add), reads=[b_sq], writes=[b_hs])
                op("act", lambda e: e.activation(hs[:, 1, :], hs[:, 0, :], AF.Ln, bias=epsc[:, 0:1], scale=1.0 / 64.0), reads=[b_hs], writes=[b_hs])
                op("act", lambda e: e.activation(hs[:, 1, :], hs[:, 1, :], AF.Exp, scale=-0.5), reads=[b_hs], writes=[b_hs])
                op("dve", lambda e: e.tensor_tensor(sq[:].rearrange("p (h e) -> p h e", e=64), o_sb[:].rearrange("p (h e) -> p h e", e=64),
                                                    hs[:, 1, :].unsqueeze(2).broadcast_to([128, 16, 64]), ALU.mult),
                   reads=[b_osb, b_hs, b_sq], writes=[b_sq])
                op("dve", lambda e: e.tensor_tensor(merged[:], sq[:], GG[b][:], ALU.mult), reads=[b_sq, b_GG[b]], writes=[b_mg])
                return None
            def s8():
                transpose8(merged, b_mg, mT, b_mT)
                for hf in range(2):
                    bk = P.balloc()
                    for k in range(8):
                        op("pe", lambda e: e.matmul(P.bank(bk), mT[:, k, :], w_out[:, k, hf * 512:(hf + 1) * 512], start=(k == 0), stop=(k == 7)),
                           reads=[b_mT, b_wout], writes=[BB[bk]])
                    op("dve", lambda e: e.tensor_tensor(hn[:, hf * 512:(hf + 1) * 512], P.bank(bk), gate_m[:, hf * 512:(hf + 1) * 512], ALU.mult),
                       reads=[BB[bk], b_mod[2]], writes=[b_hn])
                    P.bfree(bk)
                op("dve", lambda e: e.tensor_tensor(x1[b][:], hn[:], xt[b][:], ALU.add), reads=[b_hn, b_xt[b]], writes=[b_x1[b]])
                P.dma("sp", xs_d[t * 128:(t + 1) * 128, :], x1[b][:], reads=[b_x1[b]], writes=[xs_bufs[t]])
                return None
            def s9():
                rmsnorm_to_bf(x1[b], b_x1[b], st[b], b_st[b], 2, A_f, b_mod[4], shift_f, b_mod[3], False)
                transpose8(h_bf, b_hbf, h2T[b], b_h2T[b])
                P.dma("sp", h2T_d[:, :, t * 128:(t + 1) * 128], h2T[b][:], reads=[b_h2T[b]], writes=[h2T_bufs[t]])
                bk = P.balloc()
                for k in range(8):
                    op("pe", lambda e: e.matmul(P.bank(bk)[:, 0:36], h2T[b][:, k, :], Wr[:, k, :], start=(k == 0), stop=(k == 7)),
                       reads=[b_h2T[b], b_Wr], writes=[BB[bk]])
                op("dve", lambda e: e.tensor_tensor(rl[:], P.bank(bk)[:, 0:36], rbias[:], ALU.add), reads=[BB[bk], b_rbias], writes=[b_rl])
                P.bfree(bk)
                R = [b_rs]
                op("dve", lambda e: e.tensor_reduce(rs[:, 0:1], rl[:, 0:4], AX.X, ALU.max, negate=True), reads=[b_rl], writes=R)
                op("dve", lambda e: e.tensor_scalar(rs[:, 8:12], rl[:, 0:4], rs[:, 0:1], 0.0, ALU.add, ALU.is_ge), reads=[b_rl] + R, writes=R)
                op("act", lambda e: e.activation(rs[:, 12:16], rl[:, 0:4], AF.Exp, bias=rs[:, 0:1], scale=1.0, accum_out=rs[:, 1:2]),
                   reads=[b_rl] + R, writes=R)
                op("dve", lambda e: e.reciprocal(rs[:, 2:3], rs[:, 1:2]), reads=R, writes=R)
                op("dve", lambda e: e.tensor_scalar(rs[:, 8:12], rs[:, 8:12], -1.0, 1e30, ALU.add, ALU.mult), reads=R, writes=R)
                op("dve", lambda e: e.tensor_tensor(rm[:, 0, :].rearrange("p (g x) -> p g x", g=4), rl[:, 4:36].rearrange("p (g x) -> p g x", g=4),
                                                    rs[:, 8:12].unsqueeze(2).broadcast_to([128, 4, 8]), ALU.add),
                   reads=[b_rl] + R, writes=[b_rm])
                op("dve", lambda e: e.max(top8[:], rm[:, 0, :]), reads=[b_rm], writes=[b_top8])
                op("dve", lambda e: e.tensor_scalar(rm[:, 1, :], rm[:, 0, :], top8[:, 0:1], None, ALU.is_equal), reads=[b_rm, b_top8], writes=[b_rm])
                op("dve", lambda e: e.tensor_scalar(rm[:, 2, :], rm[:, 0, :], top8[:, 1:2], None, ALU.is_equal), reads=[b_rm, b_top8], writes=[b_rm])
                op("dve", lambda e: e.tensor_tensor(rs[:, 3:4], top8[:, 1:2], top8[:, 0:1], ALU.subtract), reads=[b_top8] + R, writes=R)
                op("act", lambda e: e.activation(rs[:, 4:5], rs[:, 3:4], AF.Exp), reads=R, writes=R)
                op("dve", lambda e: e.tensor_scalar(rs[:, 5:6], rs[:, 4:5], 1.0, None, ALU.add), reads=R, writes=R)
                op("dve", lambda e: e.reciprocal(rs[:, 5:6], rs[:, 5:6]), reads=R, writes=R)
                op("dve", lambda e: e.tensor_tensor(rs[:, 6:7], rs[:, 5:6], rs[:, 2:3], ALU.mult), reads=R, writes=R)
                op("dve", lambda e: e.tensor_tensor(rs[:, 7:8], rs[:, 6:7], rs[:, 4:5], ALU.mult), reads=R, writes=R)
                op("dve", lambda e: e.tensor_scalar(comb[:, t, :], rm[:, 1, :], rs[:, 6:7], None, ALU.mult), reads=[b_rm] + R, writes=[b_comb[t]])
                op("dve", lambda e: e.scalar_tensor_tensor(comb[:, t, :], rm[:, 2, :], rs[:, 7:8], comb[:, t, :], ALU.mult, ALU.add),
                   reads=[b_rm, b_comb[t]] + R, writes=[b_comb[t]])
                return None
            return dict(s12=s12, s3=s3, s4a=s4a, s4b=s4b, s5=s5, s6=s6, s7=s7, s8=s8, s9=s9)

        P.recording = SCHED
        st_prev = None
        for t in range(n_tiles + 1):
            cur = make_stages(t) if t < n_tiles else None
            seq = [(st_prev, "s5"), (cur, "s12"), (st_prev, "s6"), (cur, "s3"), (st_prev, "s7"), (cur, "s4a"),
                   (st_prev, "s8"), (cur, "s4b"), (st_prev, "s9")]
            for d_, k_ in seq:
                if d_ is not None:
                    d_[k_]()
            st_prev = cur
        if dbg and l == 0:
            P.dma("sp", comb_d, comb[:], reads=b_comb, writes=[])
        P.barrier()
        esA.close()

        if not do_moe:
            return
        esB = contextlib.ExitStack(); P.stacks.append(esB)
        NPASS = 2
        TPP = NT // NPASS
        h2ps = [sb("h2p%d" % i, [128, 8, TPP * 128], BF16, esB) for i in range(2)]; b_h2ps = [Buf("h2p0"), Buf("h2p1")]
        acc = sb("acc", [128, TPP, D], F32, esB); b_acc = [Buf("acc%d" % i) for i in range(TPP)]
        ewg = [sb("ewg%d" % i, [128, 8, DE], BF16, esB) for i in range(2)]
        ewu = [sb("ewu%d" % i, [128, 8, DE], BF16, esB) for i in range(2)]
        ewd = [sb("ewd%d" % i, [128, 2, D], BF16, esB) for i in range(2)]
        b_ew = [Buf("ew0"), Buf("ew1")]
        sgt = [sb("sgt%d" % i, [128, 512], BF16, esB) for i in range(2)]; b_sgt = [Buf("sgt0"), Buf("sgt1")]
        hid = [sb("hid%d" % i, [128, 2, 512], BF16, esB) for i in range(2)]; b_hid = [Buf("hid0"), Buf("hid1")]
        xr = [sb("xr%d" % i, [128, D], F32, esB) for i in range(2)]; b_xr = [Buf("xr0"), Buf("xr1")]
        stB = [sb("stB%d" % i, [128, 2], F32, esB) for i in range(2)]; b_stB = [Buf("stB0"), Buf("stB1")]
        junkB = sb("junkB", [128, D], BF16, esB); b_junkB = Buf("junkB")
        nfin = sb("nfin", [128, D], F32, esB); b_nfin = Buf("nfin")
        if last:
            P.dma("sp", nfin[:], nfin_d.partition_broadcast(128), writes=[b_nfin])
        BB = P.bank_bufs

        def load_expert(e_idx, slot):
            P.dma("pool", ewg[slot][:], ewg_d[l, e_idx].rearrange("(k p) f -> p k f", p=128), writes=[b_ew[slot]])
            P.dma("pool", ewu[slot][:], ewu_d[l, e_idx].rearrange("(k p) f -> p k f", p=128), writes=[b_ew[slot]])
            P.dma("pool", ewd[slot][:], ewd_d[l, e_idx].rearrange("(k p) f -> p k f", p=128), writes=[b_ew[slot]])

        P.recording = SCHED
        for pa in range(NPASS):
            h2p, b_h2p = h2ps[pa % 2], b_h2ps[pa % 2]
            P.dma("sp", h2p[:], h2T_d[:, :, pa * TPP * 128:(pa + 1) * TPP * 128],
                  reads=h2T_bufs[pa * TPP:(pa + 1) * TPP], writes=[b_h2p])
            load_expert(0, 0)
            pending = None
            blk = 0
            for ex in range(n_exp):
                slot = ex % 2
                for tb in range(TPP // 4):
                    hb = blk % 2
                    blk += 1
                    for fc in range(2):
                        bg = P.balloc()
                        for k in range(8):
                            op("pe", lambda e: e.matmul(P.bank(bg), ewg[slot][:, k, fc * 128:(fc + 1) * 128], h2p[:, k, tb * 512:(tb + 1) * 512],
                                                        start=(k == 0), stop=(k == 7)),
                               reads=[b_ew[slot], b_h2p], writes=[BB[bg]])
                        bu = P.balloc()
                        for k in range(8):
                            op("pe", lambda e: e.matmul(P.bank(bu), ewu[slot][:, k, fc * 128:(fc + 1) * 128], h2p[:, k, tb * 512:(tb + 1) * 512],
                                                        start=(k == 0), stop=(k == 7)),
                               reads=[b_ew[slot], b_h2p], writes=[BB[bu]])
                        op("act", lambda e: e.activation(sgt[fc][:], P.bank(bg), AF.Silu), reads=[BB[bg]], writes=[b_sgt[fc]])
                        P.bfree(bg)
                        op("dve", lambda e: e.tensor_tensor(hid[hb][:, fc, :], sgt[fc][:], P.bank(bu), ALU.mult),
                           reads=[b_sgt[fc], BB[bu]], writes=[b_hid[hb]])
                        P.bfree(bu)
                    if pending is not None:
                        pending()
                    if tb == 0 and ex + 1 < n_exp:
                        load_expert(ex + 1, 1 - slot)

                    def down(ex=ex, slot=slot, tb=tb, hb=hb):
                        for tt in range(4):
                            ti = tb * 4 + tt
                            for dh in range(2):
                                bd = P.balloc()
                                for fc in range(2):
                                    op("pe", lambda e: e.matmul(P.bank(bd), hid[hb][:, fc, tt * 128:(tt + 1) * 128],
                                                                ewd[slot][:, fc, dh * 512:(dh + 1) * 512], start=(fc == 0), stop=(fc == 1)),
                                       reads=[b_hid[hb], b_ew[slot]], writes=[BB[bd]])
                                cw = comb[:, pa * TPP + ti, ex:ex + 1]
                                a_ = acc[:, ti, dh * 512:(dh + 1) * 512]
                                if ex == 0:
                                    op("dve", lambda e: e.tensor_scalar(a_, P.bank(bd), cw, None, ALU.mult),
                                       reads=[BB[bd], b_comb[pa * TPP + ti]], writes=[b_acc[ti]])
                                else:
                                    op("dve", lambda e: e.scalar_tensor_tensor(a_, P.bank(bd), cw, a_, ALU.mult, ALU.add),
                                       reads=[BB[bd], b_comb[pa * TPP + ti], b_acc[ti]], writes=[b_acc[ti]])
                                P.bfree(bd)
                    pending = down
            pending()
            for ti in range(TPP):
                t = pa * TPP + ti
                b = ti % 2
                P.dma("sp", xr[b][:], xs_d[t * 128:(t + 1) * 128, :], reads=[xs_bufs[t]], writes=[b_xr[b]])
                a_t = acc[:, ti, :]
                op("dve", lambda e: e.tensor_tensor(a_t, a_t, gate_f, ALU.mult), reads=[b_acc[ti], b_mod[5]], writes=[b_acc[ti]])
                op("dve", lambda e: e.tensor_tensor(xr[b][:], a_t, xr[b][:], ALU.add), reads=[b_acc[ti], b_xr[b]], writes=[b_xr[b]])
                if not last:
                    P.dma("sp", xs_d[t * 128:(t + 1) * 128, :], xr[b][:], reads=[b_xr[b]], writes=[xs_bufs[t]])
                else:
                    op("act", lambda e: e.activation(junkB[:], xr[b][:], AF.Square, accum_out=stB[b][:, 0:1]),
                       reads=[b_xr[b]], writes=[b_junkB, b_stB[b]])
                    op("act", lambda e: e.activation(stB[b][:, 1:2], stB[b][:, 0:1], AF.Ln, bias=epsc[:, 0:1], scale=1.0 / D),
                       reads=[b_stB[b]], writes=[b_stB[b]])
                    op("act", lambda e: e.activation(stB[b][:, 1:2], stB[b][:, 1:2], AF.Exp, scale=-0.5), reads=[b_stB[b]], writes=[b_stB[b]])
                    op("dve", lambda e: e.scalar_tensor_tensor(a_t, xr[b][:], stB[b][:, 1:2], nfin[:], ALU.mult, ALU.mult),
                       reads=[b_xr[b], b_stB[b], b_nfin, b_acc[ti]], writes=[b_acc[ti]])
                    P.dma("sp", out_d[t * 128:(t + 1) * 128, :], a_t, reads=[b_acc[ti]], writes=[out_bufs[t]])
        P.barrier()
        esB.close()

    try:
        for l in range(n_layers):
            layer(l, last=(l == n_layers - 1))
    except _Stop:
        for es_ in reversed(P.stacks):
            es_.close()
    P.finish("sp")
    P.barrier()
    P.close()
    return P


_CACHE = {}


def _in_maps(inputs):
    consts = _host_consts()
    shared = {}
    for k in ["w_ada", "b_ada", "norm_mix", "norm_ffn", "w_in", "ret_norm", "hgrn_norm", "hgrn_lb_logits", "gla_wa2", "gla_ba",
              "gla_norm", "w_out", "router_group_w", "router_group_b", "router_expert_w", "router_expert_b",
              "expert_w_gate", "expert_w_up", "expert_w_down", "norm_final"]:
        shared[k] = np.ascontiguousarray(np.asarray(inputs[k], dtype=np.float32))
    shared.update(consts)
    lbl = shared["hgrn_lb_logits"].reshape(DEPTH, 3, 128).transpose(0, 2, 1)
    shared["hgrn_lb_logits"] = np.ascontiguousarray(lbl)
    wa2 = shared["gla_wa2"]
    wa2p = np.zeros((DEPTH, 16, 256), np.float32)
    ba = shared["gla_ba"]
    bap = np.zeros((DEPTH, 128, 2), np.float32)
    for h in range(4):
        wa2p[:, :, h * 64:h * 64 + 32] = wa2[:, :, h * 32:(h + 1) * 32]
        pair, half = h // 2, h % 2
        bap[:, half * 64:half * 64 + 32, pair] = ba[:, h * 32:(h + 1) * 32]
    shared["gla_wa2"] = wa2p
    shared["gla_ba"] = bap
    x = np.asarray(inputs["x"], dtype=np.float32)
    c = np.asarray(inputs["c"], dtype=np.float32)
    pos = np.asarray(inputs["positions"], dtype=np.int32)
    maps = []
    for b in range(8):
        m = dict(shared)
        m["x"] = np.ascontiguousarray(x[b])
        m["c"] = np.ascontiguousarray(c[b].reshape(8, 128).T)
        m["pos"] = np.ascontiguousarray(pos[b].reshape(NT, 128).T)
        maps.append(m)
    return maps


def kernel(**inputs):
    if "prog" not in _CACHE:
        _CACHE["prog"] = build()
    P = _CACHE["prog"]
    maps = _in_maps(inputs)
    res = run_bass_kernel_spmd(P.nc, maps, core_ids=list(range(8)))
    out = np.stack([np.asarray(r["out"], dtype=np.float32) for r in res.results], axis=0)
    return out
```

```python
import contextlib
import math
import numpy as np
import concourse.bass as bass
import concourse.mybir as mybir
from concourse.bass_utils import run_bass_kernel_spmd

F32 = mybir.dt.float32
BF16 = mybir.dt.bfloat16
I32 = mybir.dt.int32
AF = mybir.ActivationFunctionType
ALU = mybir.AluOpType
AX = mybir.AxisListType

D = 1024
S = 4096
NT = S // 128
DEPTH = 2
NE = 32
DE = 256
EPS = 1e-6
NCOL = 4112
W_IN_MAP = [
    (0, 384, 0, False),
    (384, 768, 384, False),
    (768, 1152, 768, False),
    (2304, 2688, 1152, False),
    (3328, 3584, 1536, False),
    (1152, 1536, 1792, False),
    (2688, 3072, 2176, False),
    (3584, 3840, 2560, False),
    (1536, 1920, 2816, False),
    (1920, 2304, 3200, False),
    (3072, 3200, 3584, True),
    (3200, 3328, 3840, True),
    (3840, 3856, 4096, False),
]
C_V = 768
C_G = 1792
C_HQ = 2816
C_HF = 3200
C_GQ = 3584
C_GK = 3840
C_GA = 4096


class Buf:
    __slots__ = ("name", "last_w", "readers")

    def __init__(self, name=""):
        self.name = name
        self.last_w = None
        self.readers = {}


class _Rec:
    def __init__(self):
        self.call = None

    def __getattr__(self, name):
        def f(*a, **kw):
            self.call = (name, a, kw)
            return self
        return f


def _free_elems(ap):
    n = 1
    for d_ in ap.shape[1:]:
        n *= d_
    return n


class Prog:
    N_DMA_SEMS = 24

    def __init__(self):
        self.nc = bass.Bass("TRN2", target_bir_lowering=False)
        nc = self.nc
        self.es = contextlib.ExitStack()
        self.eng = {"pe": nc.tensor, "act": nc.scalar, "dve": nc.vector, "pool": nc.gpsimd, "sp": nc.sync}
        self.sem = {}
        self.cnt = {}
        for e in self.eng:
            self.sem[e] = self.es.enter_context(nc.semaphore("s_" + e))
            self.cnt[e] = 0
        self.dma_sems = {}
        self.dma_rr = {}
        for q, n in (("sp", 16), ("pool", 16), ("act", 4)):
            self.dma_sems[q] = []
            self.dma_rr[q] = 0
            for j in range(n):
                k = ("dma", q, j)
                self.sem[k] = self.es.enter_context(nc.semaphore("s_dma_%s%d" % (q, j)))
                self.cnt[k] = 0
                self.dma_sems[q].append(k)
        self.waited = {e: {} for e in self.eng}
        self.n_wait = 0
        self.n_ins = 0
        self.stacks = []
        self.recording = False
        self.pending = []
        self.psum = self.es.enter_context(nc.psum_tensor("psum_all", [128, 4096], F32))
        self.bank_bufs = [Buf("bank%d" % i) for i in range(8)]
        self.bank_free = list(range(8))

    def sb(self, name, shape, dt, es=None):
        self.n_sb = getattr(self, "n_sb", 0) + 1
        return (es or self.es).enter_context(self.nc.sbuf_tensor("sb%d_%s" % (self.n_sb, name), list(shape), dt))

    def balloc(self):
        assert self.bank_free, "out of PSUM banks"
        i = self.bank_free.pop(0)
        return i

    def bfree(self, i):
        self.bank_free.append(i)

    def bank(self, i):
        return self.psum[:, i * 512:(i + 1) * 512]

    def bank_bf(self, i):
        return self.psum[:, i * 512:(i + 1) * 512].bitcast(BF16)

    def _wait(self, e, sk, v):
        if v <= 0:
            return
        if self.waited[e].get(sk, 0) >= v:
            return
        if sk == e and (e == "pe" or e == "sp"):
            return
        self.eng[e].wait_ge(self.sem[sk], v)
        self.waited[e][sk] = v
        self.n_wait += 1

    def _deps(self, e, reads, writes):
        for b in reads:
            if b.last_w is not None:
                self._wait(e, *b.last_w)
        for b in writes:
            if b.last_w is not None:
                self._wait(e, *b.last_w)
            for sk, v in b.readers.items():
                self._wait(e, sk, v)

    def _mark(self, tok, reads, writes):
        sk, v = tok
        for b in reads:
            if b.readers.get(sk, 0) < v:
                b.readers[sk] = v
        for b in writes:
            b.last_w = tok
            b.readers = {}

    def _cost(self, it):
        if it["kind"] == "dma":
            return 0.15, 2.5
        name, a, kw = it["call"]
        e = it["e"]
        out = a[0] if a else kw.get("out")
        try:
            n = _free_elems(out)
        except Exception:
            n = 256
        if e == "pe":
            c = 0.03 + n * 0.00045
        elif e == "act":
            c = 0.22 + n * 0.00105
        elif e == "dve":
            c = 0.13 + n * (0.0021 if name == "tensor_tensor_scan" else 0.00105)
        else:
            c = 0.35 + n * 0.0021
        return c, c

    def flush(self):
        items, self.pending = self.pending, []
        self.recording = False
        n = len(items)
        if n == 0:
            return
        lastw, readers = {}, {}
        last_pe = [None]
        preds = [set() for _ in range(n)]
        for i, it in enumerate(items):
            for b in it["reads"]:
                k = id(b)
                if k in lastw:
                    preds[i].add(lastw[k])
            for b in it["writes"]:
                k = id(b)
                if k in lastw:
                    preds[i].add(lastw[k])
                for r in readers.get(k, ()):
                    preds[i].add(r)
            for b in it["reads"]:
                readers.setdefault(id(b), []).append(i)
            for b in it["writes"]:
                lastw[id(b)] = i
                readers[id(b)] = []
            preds[i].discard(i)
            if KEEP_PE_ORDER and it["e"] == "pe":
                if last_pe[0] is not None:
                    preds[i].add(last_pe[0])
                last_pe[0] = i
        succs = [[] for _ in range(n)]
        indeg = [0] * n
        for i in range(n):
            indeg[i] = len(preds[i])
            for p in preds[i]:
                succs[p].append(i)
        ready = [i for i in range(n) if indeg[i] == 0]
        eng_free = {}
        finish = [0.0] * n
        est = [0.0] * n
        order = []
        LAT = 0.3
        WIN = 800
        lo = 0
        done = [False] * n
        while ready:
            while lo < n and done[lo]:
                lo += 1
            best, bkey = None, None
            for i in ready:
                if i > lo + WIN:
                    continue
                q = items[i]["e"]
                stt = max(eng_free.get(q, 0.0), est[i])
                key = (stt, i)
                if bkey is None or key < bkey:
                    best, bkey = i, key
            if best is None:
                best = min(ready)
                bkey = (max(eng_free.get(items[best]["e"], 0.0), est[best]), best)
            ready.remove(best)
            busy, lat = self._cost(items[best])
            q = items[best]["e"]
            eng_free[q] = bkey[0] + busy
            finish[best] = bkey[0] + lat
            done[best] = True
            order.append(best)
            for s_ in succs[best]:
                if items[s_]["e"] == q:
                    lat_ = 0.0 if q == "pe" else 0.1
                else:
                    lat_ = LAT
                est[s_] = max(est[s_], finish[best] + lat_)
                indeg[s_] -= 1
                if indeg[s_] == 0:
                    ready.append(s_)
        assert len(order) == n
        for i in order:
            it = items[i]
            if it["kind"] == "dma":
                self.dma(it["e"], it["out"], it["in_"], it["reads"], it["writes"], **it["kw"])
            else:
                name, a, kw = it["call"]
                self.op(it["e"], lambda eng, name=name, a=a, kw=kw: getattr(eng, name)(*a, **kw), it["reads"], it["writes"])

    def op(self, e, fn, reads=(), writes=()):
        if self.recording:
            r = _Rec()
            fn(r)
            self.pending.append(dict(kind="op", e=e, call=r.call, reads=list(reads), writes=list(writes)))
            return None
        self._deps(e, reads, writes)
        ins = fn(self.eng[e])
        self.cnt[e] += 1
        ins.then_inc(self.sem[e], 1)
        self._mark((e, self.cnt[e]), reads, writes)
        self.n_ins += 1
        return ins

    def dma(self, q, out, in_, reads=(), writes=(), **kw):
        if self.recording:
            self.pending.append(dict(kind="dma", e=q, out=out, in_=in_, kw=kw, reads=list(reads), writes=list(writes)))
            return None
        k = self.dma_sems[q][self.dma_rr[q]]
        self.dma_rr[q] = (self.dma_rr[q] + 1) % len(self.dma_sems[q])
        self._wait(q, k, self.cnt[k])
        self._deps(q, reads, writes)
        ins = self.eng[q].dma_start(out=out, in_=in_, **kw)
        self.cnt[k] += 16
        ins.then_inc(self.sem[k], 16)
        self._mark((k, self.cnt[k]), reads, writes)
        self.n_ins += 1
        return ins

    def barrier(self):
        if self.recording or self.pending:
            self.flush()
        for e in self.eng:
            for sk in self.sem:
                if sk != e:
                    self._wait(e, sk, self.cnt[sk])

    def finish(self, e="sp"):
        if self.recording or self.pending:
            self.flush()
        for sk in self.sem:
            if sk != e:
                self._wait(e, sk, self.cnt[sk])

    def close(self):
        self.es.close()


def _host_consts():
    inv_freq = (10000.0 ** (-np.arange(0, 64, 2, dtype=np.float32) / np.float32(64))).astype(np.float32)
    invf = np.broadcast_to(inv_freq[None, :], (128, 32)).astype(np.float32).copy()
    h = np.arange(6, dtype=np.float64)
    lg = np.log1p(-np.exp2(-5.0 - h))
    pos = (np.arange(128) % 64).astype(np.float64)
    xq = np.exp(lg[None, :] * (pos[:, None] - 31.0))
    xk = np.exp(lg[None, :] * (31.0 - pos[:, None])) * (64.0 ** -0.5)
    XQK = np.concatenate([np.repeat(xq, 64, axis=1), np.repeat(xk, 64, axis=1)], axis=1).astype(np.float32)
    cret = np.zeros((128, 3), np.float64)
    for p in range(3):
        for half in range(2):
            cret[half * 64:(half + 1) * 64, p] = np.exp(lg[2 * p + half] * 64.0)
    CRET2 = np.repeat(cret[:, :, None], 2, axis=2).astype(np.float32)
    return {"invf": invf, "xqk": XQK, "cret": CRET2.reshape(128, 6)}


class _Stop(Exception):
    pass


SCHED = True
KEEP_PE_ORDER = False


def build(n_layers=DEPTH, do_moe=True, dbg=False, n_tiles=NT, n_exp=NE, cut=0):
    P = Prog()

    def ck(k):
        if cut == k:
            raise _Stop()

    nc = P.nc
    op = P.op

    def din(name, shape, dt=F32):
        return nc.dram_tensor(name, list(shape), dt, kind="ExternalInput").ap()

    x_d = din("x", [S, D])
    c_d = din("c", [128, 8])
    pos_d = din("pos", [128, NT], I32)
    w_ada_d = din("w_ada", [DEPTH, D, 6 * D])
    b_ada_d = din("b_ada", [DEPTH, 6 * D])
    norm_mix_d = din("norm_mix", [DEPTH, D])
    norm_ffn_d = din("norm_ffn", [DEPTH, D])
    w_in_d = din("w_in", [DEPTH, D, 3856])
    ret_norm_d = din("ret_norm", [DEPTH, 384])
    hgrn_norm_d = din("hgrn_norm", [DEPTH, 384])
    lbl_d = din("hgrn_lb_logits", [DEPTH, 128, 3])
    wa2_d = din("gla_wa2", [DEPTH, 16, 256])
    gba_d = din("gla_ba", [DEPTH, 128, 2])
    gla_norm_d = din("gla_norm", [DEPTH, 256])
    w_out_d = din("w_out", [DEPTH, D, D])
    rgw_d = din("router_group_w", [DEPTH, D, 4])
    rgb_d = din("router_group_b", [DEPTH, 4])
    rew_d = din("router_expert_w", [DEPTH, D, 32])
    reb_d = din("router_expert_b", [DEPTH, 32])
    ewg_d = din("expert_w_gate", [DEPTH, NE, D, DE])
    ewu_d = din("expert_w_up", [DEPTH, NE, D, DE])
    ewd_d = din("expert_w_down", [DEPTH, NE, DE, D])
    nfin_d = din("norm_final", [D])
    invf_d = din("invf", [128, 32])
    xqk_d = din("xqk", [128, 768])
    cret_d = din("cret", [128, 6])
    out_d = nc.dram_tensor("out", [S, D], F32, kind="ExternalOutput").ap()
    skind = "ExternalOutput" if dbg else "Internal"
    xs_d = nc.dram_tensor("xs", [S, D], F32, kind=skind).ap()
    h2T_d = nc.dram_tensor("h2T", [128, 8, S], BF16, kind=skind).ap()
    comb_d = nc.dram_tensor("combd", [128, NT, 32], F32, kind=skind).ap() if dbg else None

    xs_bufs = [Buf("xs%d" % t) for t in range(NT)]
    h2T_bufs = [Buf("h2T%d" % t) for t in range(NT)]
    out_bufs = [Buf("out%d" % t) for t in range(NT)]

    sb = P.sb
    ident = sb("ident", [128, 128], BF16); b_ident = Buf("ident")
    mask4 = sb("mask4", [128, 4, 128], I32); b_mask = Buf("mask4")
    ones = sb("ones", [128, 128], F32); b_ones = Buf("ones")
    zeros = sb("zeros", [128, 8], F32); b_zeros = Buf("zeros")
    epsc = sb("epsc", [128, 1], F32); b_epsc = Buf("epsc")
    COS = sb("COS", [128, NT, 32], BF16); b_cos = Buf("COS")
    SIN = sb("SIN", [128, NT, 32], BF16); b_sin = Buf("SIN")
    XQK = sb("XQK", [128, 768], F32); b_xqk = Buf("XQK")
    cact = sb("cact", [128, 8], F32); b_cact = Buf("cact")
    mod = sb("mod", [128, 6, D], BF16); b_mod = [Buf("mod%d" % i) for i in range(6)]
    comb = sb("comb", [128, NT, 32], F32); b_comb = [Buf("comb%d" % t) for t in range(NT)]
    gain = sb("gain", [128, D], BF16); b_gain = Buf("gain")
    rbias = sb("rbias", [128, 36], F32); b_rbias = Buf("rbias")
    Wr = sb("Wr", [128, 8, 36], BF16); b_Wr = Buf("Wr")
    lbT = sb("lbT", [128, 2, 3], F32); b_lb = Buf("lbT")
    wa2p = sb("wa2p", [16, 256], BF16); b_wa2 = Buf("wa2p")
    nba = sb("nba", [128, 2], F32); b_nba = Buf("nba")
    cvec = [sb("cvec%d" % i, [128, 8, 2], F32) for i in range(2)]; b_cvec = [Buf("cvec0"), Buf("cvec1")]

    op("pool", lambda e: e.memset(ident[:], 0.0), writes=[b_ident])
    op("pool", lambda e: e.affine_select(ident[:], ident[:], [[-1, 128]], ALU.not_equal, 1.0, base=0, channel_multiplier=1),
       reads=[b_ident], writes=[b_ident])
    op("pool", lambda e: e.memset(mask4[:], 1), writes=[b_mask])
    for g in range(4):
        op("pool", lambda e: e.affine_select(mask4[:, g, :], mask4[:, g, :], [[1, 128]], ALU.is_ge, 0.0, base=0, channel_multiplier=-1),
           reads=[b_mask], writes=[b_mask])
        op("pool", lambda e: e.memset(mask4[0:64, g, 64:128], 0), reads=[b_mask], writes=[b_mask])
    if dbg:
        op("pool", lambda e: e.memset(comb[:], 0.0), writes=b_comb)
    op("pool", lambda e: e.memset(ones[:], 1.0), writes=[b_ones])
    op("pool", lambda e: e.memset(zeros[:], 0.0), writes=[b_zeros])
    op("pool", lambda e: e.memset(epsc[:], EPS), writes=[b_epsc])
    P.dma("sp", XQK[:], xqk_d, writes=[b_xqk])
    for i in range(2):
        P.dma("sp", cvec[i][:, 0:3, :], cret_d.rearrange("p (a b) -> p a b", a=3), writes=[b_cvec[i]])

    def setup_tables(es0):
        c_sb = sb("c_sb", [128, 8], F32, es0); b_c = Buf("c")
        P.dma("sp", c_sb[:], c_d, writes=[b_c])
        op("act", lambda e: e.activation(c_sb[:], c_sb[:], AF.Silu), reads=[b_c], writes=[b_c])
        op("dve", lambda e: e.tensor_copy(cact[:], c_sb[:]), reads=[b_c], writes=[b_cact])
        pos_i = sb("pos_i", [128, NT], I32, es0); b_pi = Buf("pos_i")
        pos_f = sb("pos_f", [128, NT], F32, es0); b_pf = Buf("pos_f")
        invf = sb("invf", [128, 32], F32, es0); b_if = Buf("invf")
        ang = sb("ang", [128, NT, 32], F32, es0); b_ang = Buf("ang")
        kk_i = sb("kk_i", [128, NT, 32], I32, es0); b_kki = Buf("kki")
        kk = sb("kk", [128, NT, 32], F32, es0); b_kk = Buf("kk")
        rr = sb("rr", [128, NT, 32], F32, es0); b_rr = Buf("rr")
        mm = sb("mm", [128, NT, 32], F32, es0); b_mm = Buf("mm")
        P.dma("sp", pos_i[:], pos_d, writes=[b_pi])
        P.dma("sp", invf[:], invf_d, writes=[b_if])
        op("dve", lambda e: e.tensor_copy(pos_f[:], pos_i[:]), reads=[b_pi], writes=[b_pf])
        op("dve", lambda e: e.tensor_tensor(ang[:], pos_f[:].unsqueeze(2).broadcast_to([128, NT, 32]),
                                            invf[:].unsqueeze(1).broadcast_to([128, NT, 32]), ALU.mult),
           reads=[b_pf, b_if], writes=[b_ang])
        TWO_PI = 2.0 * math.pi
        HI = 6.28125
        LO = TWO_PI - HI

        def wrap(dst_tab, shift):
            op("dve", lambda e: e.tensor_scalar(rr[:], ang[:], shift, 1.0 / TWO_PI, ALU.add, ALU.mult), reads=[b_ang], writes=[b_rr])
            op("dve", lambda e: e.tensor_copy(kk_i[:], rr[:]), reads=[b_rr], writes=[b_kki])
            op("dve", lambda e: e.tensor_copy(kk[:], kk_i[:]), reads=[b_kki], writes=[b_kk])
            op("dve", lambda e: e.scalar_tensor_tensor(rr[:], kk[:], -HI, ang[:], ALU.mult, ALU.add), reads=[b_kk, b_ang], writes=[b_rr])
            op("dve", lambda e: e.scalar_tensor_tensor(rr[:], kk[:], -LO, rr[:], ALU.mult, ALU.add), reads=[b_kk, b_rr], writes=[b_rr])
            if shift != 0.0:
                op("dve", lambda e: e.tensor_scalar(rr[:], rr[:], shift, None, ALU.add), reads=[b_rr], writes=[b_rr])
            op("dve", lambda e: e.tensor_single_scalar(mm[:], rr[:], math.pi, ALU.is_gt), reads=[b_rr], writes=[b_mm])
            op("dve", lambda e: e.scalar_tensor_tensor(rr[:], mm[:], -TWO_PI, rr[:], ALU.mult, ALU.add), reads=[b_mm, b_rr], writes=[b_rr])
            op("dve", lambda e: e.tensor_single_scalar(mm[:], rr[:], -math.pi, ALU.is_lt), reads=[b_rr], writes=[b_mm])
            op("dve", lambda e: e.scalar_tensor_tensor(rr[:], mm[:], TWO_PI, rr[:], ALU.mult, ALU.add), reads=[b_mm, b_rr], writes=[b_rr])
            op("dve", lambda e: e.tensor_scalar(rr[:], rr[:], 3.1415925, -3.1415925, ALU.min, ALU.max), reads=[b_rr], writes=[b_rr])
            return op("act", lambda e: e.activation(dst_tab[0][:], rr[:], AF.Sin), reads=[b_rr], writes=[dst_tab[1]])

        wrap((SIN, b_sin), 0.0)
        wrap((COS, b_cos), math.pi / 2.0)

    def layer(l, last):
        esA = contextlib.ExitStack(); P.stacks.append(esA)
        w_in = sb("w_in", [128, 8, NCOL], BF16, esA); b_win = Buf("w_in")
        w_out = sb("w_out", [128, 8, D], BF16, esA); b_wout = Buf("w_out")

        op("pool", lambda e: e.memset(w_in[:, :, C_GQ:C_GA], 0.0), writes=[b_win])
        for (slo, shi, dlo, pad) in W_IN_MAP:
            src = w_in_d[l, :, slo:shi].rearrange("(k p) f -> p k f", p=128)
            if not pad:
                P.dma("pool", w_in[:, :, dlo:dlo + (shi - slo)], src, reads=[], writes=[b_win])
            else:
                for h in range(4):
                    P.dma("pool", w_in[:, :, dlo + h * 64:dlo + h * 64 + 32],
                          w_in_d[l, :, slo + h * 32:slo + (h + 1) * 32].rearrange("(k p) f -> p k f", p=128), writes=[b_win])
        P.dma("pool", w_out[:], w_out_d[l].rearrange("(k p) f -> p k f", p=128), writes=[b_wout])
        P.dma("pool", Wr[:, :, 0:4], rgw_d[l].rearrange("(k p) f -> p k f", p=128), writes=[b_Wr])
        P.dma("pool", Wr[:, :, 4:36], rew_d[l].rearrange("(k p) f -> p k f", p=128), writes=[b_Wr])
        P.dma("sp", rbias[:, 0:4], rgb_d[l].partition_broadcast(128), writes=[b_rbias])
        P.dma("sp", rbias[:, 4:36], reb_d[l].partition_broadcast(128), writes=[b_rbias])
        P.dma("pool", gain[:, 0:384], ret_norm_d[l].partition_broadcast(128), writes=[b_gain])
        P.dma("pool", gain[:, 384:768], hgrn_norm_d[l].partition_broadcast(128), writes=[b_gain])
        P.dma("pool", gain[:, 768:1024], gla_norm_d[l].partition_broadcast(128), writes=[b_gain])
        if l == 0:
            op("pool", lambda e: e.memset(lbT[:, 0, :], 0.0), writes=[b_lb])
            op("pool", lambda e: e.memset(lbT[:, 1, :], 0.0), reads=[b_lb], writes=[b_lb])
        else:
            P.dma("sp", lbT[:, 0, :], lbl_d[0], writes=[b_lb])
            P.dma("sp", lbT[:, 1, :], lbl_d[1], writes=[b_lb])
            op("dve", lambda e: e.tensor_tensor(lbT[:, 0, :], lbT[:, 1, :], lbT[:, 0, :], ALU.subtract), reads=[b_lb], writes=[b_lb])
            op("act", lambda e: e.activation(lbT[:, 0, :], lbT[:, 0, :], AF.Sigmoid), reads=[b_lb], writes=[b_lb])
            op("dve", lambda e: e.tensor_scalar(lbT[:, 1, :], lbT[:, 0, :], -1.0, 1.0, ALU.mult, ALU.add), reads=[b_lb], writes=[b_lb])
            op("act", lambda e: e.activation(lbT[:, 1, :], lbT[:, 1, :], AF.Ln), reads=[b_lb], writes=[b_lb])
        P.dma("pool", wa2p[:], wa2_d[l], writes=[b_wa2])
        P.dma("sp", nba[:], gba_d[l], writes=[b_nba])
        op("dve", lambda e: e.tensor_scalar(nba[:], nba[:], -1.0, None, ALU.mult), reads=[b_nba], writes=[b_nba])

        with contextlib.ExitStack() as es1:
            P.stacks.append(es1)
            if l == 0:
                setup_tables(es1)
            wad = [sb("wad%d" % i, [128, 8, 256], BF16, es1) for i in range(2)]
            b_wad = [Buf("wad0"), Buf("wad1")]
            wst = [sb("wst%d" % i, [128, 8, 256], F32, es1) for i in range(2)]
            b_wst = [Buf("wst0"), Buf("wst1")]
            bad = sb("bad", [128, 6 * D], F32, es1); b_bad = Buf("bad")
            gmx = sb("gmx", [128, 2, D], F32, es1); b_gmx = Buf("gmx")
            P.dma("sp", bad[:], b_ada_d[l].partition_broadcast(128), writes=[b_bad])
            P.dma("sp", gmx[:, 0, :], norm_mix_d[l].partition_broadcast(128), writes=[b_gmx])
            P.dma("sp", gmx[:, 1, :], norm_ffn_d[l].partition_broadcast(128), writes=[b_gmx])
            crep = sb("crep", [128, 8, 128], BF16, es1); b_crep = Buf("crep")
            op("dve", lambda e: e.tensor_copy(crep[:], cact[:].unsqueeze(2).broadcast_to([128, 8, 128])), reads=[b_cact], writes=[b_crep])
            for cb in range(24):
                w = wad[cb % 2]; bw = b_wad[cb % 2]
                ws_, bws_ = wst[cb % 2], b_wst[cb % 2]
                P.dma("sp", ws_[:], w_ada_d[l, :, cb * 256:(cb + 1) * 256].rearrange("(k p) f -> p k f", p=128), writes=[bws_])
                if cb % 2 == 0:
                    op("act", lambda e: e.copy(w[:], ws_[:]), reads=[bws_], writes=[bw])
                else:
                    op("dve", lambda e: e.tensor_copy(w[:], ws_[:]), reads=[bws_], writes=[bw])
                bk = P.balloc()
                for k in range(8):
                    op("pe", lambda e: e.matmul(P.bank(bk)[:, 0:256], crep[:, k, :], w[:, k, :], start=(k == 0), stop=(k == 7)),
                       reads=[b_crep, bw], writes=[P.bank_bufs[bk]])
                op("dve", lambda e: e.tensor_tensor(bad[:, cb * 256:(cb + 1) * 256], P.bank(bk)[:, 0:256], bad[:, cb * 256:(cb + 1) * 256], ALU.add),
                   reads=[P.bank_bufs[bk], b_bad], writes=[b_bad])
                P.bfree(bk)
            op("dve", lambda e: e.scalar_tensor_tensor(bad[:, D:2 * D], bad[:, D:2 * D], 1.0, gmx[:, 0, :], ALU.add, ALU.mult),
               reads=[b_bad, b_gmx], writes=[b_bad])
            op("dve", lambda e: e.scalar_tensor_tensor(bad[:, 4 * D:5 * D], bad[:, 4 * D:5 * D], 1.0, gmx[:, 1, :], ALU.add, ALU.mult),
               reads=[b_bad, b_gmx], writes=[b_bad])
            op("act", lambda e: e.copy(mod[:, 0:3, :].rearrange("p a f -> p (a f)"), bad[:, 0:3 * D]), reads=[b_bad], writes=b_mod[0:3])
            op("dve", lambda e: e.tensor_copy(mod[:, 3:6, :].rearrange("p a f -> p (a f)"), bad[:, 3 * D:6 * D]), reads=[b_bad], writes=b_mod[3:6])
            P.barrier()
        shift_m, A_m, gate_m, shift_f, A_f, gate_f = [mod[:, i, :] for i in range(6)]
        ck(2)

        def two(name, shape, dt):
            return [sb("%s%d" % (name, i), shape, dt, esA) for i in range(2)], [Buf("%s%d" % (name, i)) for i in range(2)]
        xt, b_xt = two("xt", [128, D], F32)
        st, b_st = two("st", [128, 4], F32)
        hn = sb("hn", [128, D], F32, esA); b_hn = Buf("hn")
        h_bf = sb("h_bf", [128, D], BF16, esA); b_hbf = Buf("h_bf")
        hnA = sb("hnA", [128, D], F32, esA); b_hnA = Buf("hnA")
        h_bfA = sb("h_bfA", [128, D], BF16, esA); b_hbfA = Buf("h_bfA")
        def one(name, shape, dt):
            t_ = sb(name, shape, dt, esA); b_ = Buf(name)
            return [t_, t_], [b_, b_]
        hT, b_hT = one("hT", [128, 8, 128], BF16)
        qk_s = sb("qk_s", [128, 768], F32, esA); b_qks = Buf("qk_s")
        rt = [sb("rt%d" % i, [128, 12, 32], F32, esA) for i in range(4)]; b_rt = [Buf("rt%d" % i) for i in range(4)]
        QK, b_QK = two("QK", [128, 384 + 1024], BF16)
        QKT, b_QKT = two("QKT", [128, 2, 8, 128], BF16)
        Vall, b_V = two("Vall", [128, D], BF16)
        GG, b_GG = two("GG", [128, D], BF16)
        ff = sb("ff", [128, 3, 128], F32, esA); b_ff = Buf("ff")
        L1 = sb("L1", [128, 3, 128], F32, esA); b_L1 = Buf("L1")
        silq = sb("silq", [128, 3, 128], F32, esA); b_silq = Buf("silq")
        lf = sb("lf", [128, 5, 128], F32, esA); b_lf = Buf("lf")
        Gc, b_G = two("Gc", [128, 5, 128], F32)
        Ee, b_E = lf, b_lf
        qfac = sb("qfac", [128, 5, 128], F32, esA); b_qf = Buf("qfac")
        kfac = sb("kfac", [128, 5, 128], F32, esA); b_kf = Buf("kfac")
        dd = sb("dd", [128, 5, 2], F32, esA); b_dd = Buf("dd")
        gaT = sb("gaT", [16, 128], BF16, esA); b_gaT = Buf("gaT")
        exg = sb("exg", [128, 2, 128], F32, esA); b_exg = Buf("exg")
        PT, b_PT = one("PT", [128, 16, 128], BF16)
        Sbf = sb("Sbf", [128, 2, 8, 128], BF16, esA); b_Sbf = [Buf("Sbf0"), Buf("Sbf1")]
        Wst = sb("Wst", [128, 8, 128], F32, esA); b_W = Buf("W")
        tmpW, b_tW = hn[:].rearrange("p (a d) -> p a d", a=8), b_hn
        o_sb = sb("o_sb", [128, D], F32, esA); b_osb = Buf("o_sb")
        sq, b_sq = hn, b_hn
        hs = sb("hs", [128, 2, 16], F32, esA); b_hs = Buf("hs")
        merged = sb("merged", [128, D], BF16, esA); b_mg = Buf("merged")
        junk, b_junk = merged, b_mg
        mT = sb("mT", [128, 8, 128], BF16, esA); b_mT = Buf("mT")
        x1, b_x1 = xt, b_xt
        h2T, b_h2T = one("h2Ts", [128, 8, 128], BF16)
        rl = sb("rl", [128, 36], F32, esA); b_rl = Buf("rl")
        rs = sb("rs", [128, 16], F32, esA); b_rs = Buf("rs")
        rm = sb("rm", [128, 3, 32], F32, esA); b_rm = Buf("rm")
        top8 = sb("top8", [128, 8], F32, esA); b_top8 = Buf("top8")

        op("pool", lambda e: e.memset(Wst[:], 0.0), writes=[b_W])
        op("pool", lambda e: e.memset(PT[0][:], 0.0), writes=[b_PT[0]])

        x_src = x_d if l == 0 else xs_d
        hnB, b_hnB, h_bfB, b_hbfB, junkB_, b_junkB_ = hn, b_hn, h_bf, b_hbf, junk, b_junk

        def rmsnorm_to_bf(xin, b_xin, stt, b_stt, col, A, b_A, shift, b_shift, first):
            if first:
                hn, b_hn, h_bf, b_hbf, junk, b_junk = hnA, b_hnA, h_bfA, b_hbfA, h_bfA, b_hbfA
            else:
                hn, b_hn, h_bf, b_hbf, junk, b_junk = hnB, b_hnB, h_bfB, b_hbfB, junkB_, b_junkB_
            op("act", lambda e: e.activation(junk[:], xin[:], AF.Square, accum_out=stt[:, col:col + 1]),
               reads=[b_xin], writes=[b_junk, b_stt])
            op("act", lambda e: e.activation(stt[:, col + 1:col + 2], stt[:, col:col + 1], AF.Ln, bias=epsc[:, 0:1], scale=1.0 / D),
               reads=[b_stt], writes=[b_stt])
            op("act", lambda e: e.activation(stt[:, col + 1:col + 2], stt[:, col + 1:col + 2], AF.Exp, scale=-0.5), reads=[b_stt], writes=[b_stt])
            op("dve", lambda e: e.scalar_tensor_tensor(hn[:], xin[:], stt[:, col + 1:col + 2], A, ALU.mult, ALU.mult),
               reads=[b_xin, b_stt, b_A], writes=[b_hn])
            op("dve", lambda e: e.tensor_tensor(h_bf[:], hn[:], shift, ALU.add), reads=[b_hn, b_shift], writes=[b_hbf])

        def transpose8(src, b_src, dst, b_dst):
            bk = P.balloc()
            for k in range(8):
                op("pe", lambda e: e.transpose(P.bank_bf(bk)[:, k * 128:(k + 1) * 128], src[:, k * 128:(k + 1) * 128], ident[:]),
                   reads=[b_src, b_ident], writes=[P.bank_bufs[bk]])
            op("act", lambda e: e.copy(dst[:].rearrange("p k t -> p (k t)"), P.bank_bf(bk)), reads=[P.bank_bufs[bk]], writes=[b_dst])
            P.bfree(bk)

        def mm_tm(bk, lhs, b_lhs, c0, n):
            for k in range(8):
                op("pe", lambda e: e.matmul(P.bank(bk)[:, 0:n], lhs[:, k, :], w_in[:, k, c0:c0 + n], start=(k == 0), stop=(k == 7)),
                   reads=[b_lhs, b_win], writes=[P.bank_bufs[bk]])

        def mm_fm(bk, col, lhs, b_lhs, c0, m=128):
            for k in range(8):
                op("pe", lambda e: e.matmul(P.bank(bk)[0:m, col:col + 128], w_in[:, k, c0:c0 + m], lhs[:, k, :], start=(k == 0), stop=(k == 7)),
                   reads=[b_lhs, b_win], writes=[P.bank_bufs[bk]])

        def make_stages(t):
            b = t % 2
            pb = 1 - b
            BB = P.bank_bufs
            kTok = QK[b][:, 384:384 + 1024].rearrange("p (a d) -> p a d", a=8)
            def s12():
                P.dma("sp", xt[b][:], x_src[t * 128:(t + 1) * 128, :], reads=[xs_bufs[t]] if l > 0 else [], writes=[b_xt[b]])
                rmsnorm_to_bf(xt[b], b_xt[b], st[b], b_st[b], 0, A_m, b_mod[1], shift_m, b_mod[0], True)
                transpose8(h_bfA, b_hbfA, hT[b], b_hT[b])
                return None
            def s3():
                bq = P.balloc(); mm_tm(bq, hT[b], b_hT[b], 0, 384)
                bkk = P.balloc(); mm_tm(bkk, hT[b], b_hT[b], 384, 384)
                op("dve", lambda e: e.tensor_tensor(qk_s[:, 0:384], P.bank(bq)[:, 0:384], XQK[:, 0:384], ALU.mult),
                   reads=[BB[bq], b_xqk], writes=[b_qks])
                op("dve", lambda e: e.tensor_tensor(qk_s[:, 384:768], P.bank(bkk)[:, 0:384], XQK[:, 384:768], ALU.mult),
                   reads=[BB[bkk], b_xqk], writes=[b_qks])
                P.bfree(bq); P.bfree(bkk)
                qv = qk_s[:].rearrange("p (h two f) -> p h two f", two=2, f=32)
                x1v, x2v = qv[:, :, 0, :], qv[:, :, 1, :]
                cosb = COS[:, t, :].unsqueeze(1).broadcast_to([128, 12, 32])
                sinb = SIN[:, t, :].unsqueeze(1).broadcast_to([128, 12, 32])
                ov = QK[b][:, 0:768].rearrange("p (h two f) -> p h two f", two=2, f=32)
                op("pool", lambda e: e.tensor_tensor(rt[0][:], x1v, cosb, ALU.mult), reads=[b_qks, b_cos], writes=[b_rt[0]])
                op("pool", lambda e: e.tensor_tensor(rt[1][:], x2v, sinb, ALU.mult), reads=[b_qks, b_sin], writes=[b_rt[1]])
                op("dve", lambda e: e.tensor_tensor(ov[:, :, 0, :], rt[0][:], rt[1][:], ALU.subtract), reads=[b_rt[0], b_rt[1]], writes=[b_QK[b]])
                op("pool", lambda e: e.tensor_tensor(rt[2][:], x1v, sinb, ALU.mult), reads=[b_qks, b_sin], writes=[b_rt[2]])
                op("pool", lambda e: e.tensor_tensor(rt[3][:], x2v, cosb, ALU.mult), reads=[b_qks, b_cos], writes=[b_rt[3]])
                op("dve", lambda e: e.tensor_tensor(ov[:, :, 1, :], rt[2][:], rt[3][:], ALU.add), reads=[b_rt[2], b_rt[3]], writes=[b_QK[b]])
                bk = P.balloc()
                for j in range(6):
                    op("pe", lambda e: e.transpose(P.bank_bf(bk)[:, j * 128:(j + 1) * 128], QK[b][:, j * 128:(j + 1) * 128], ident[:]),
                       reads=[b_QK[b], b_ident], writes=[BB[bk]])
                op("act", lambda e: e.copy(QKT[b][:, :, 0:3, :].rearrange("p a c t -> p a (c t)"),
                                           P.bank_bf(bk)[:, 0:768].rearrange("p (a x) -> p a x", a=2)),
                   reads=[BB[bk]], writes=[b_QKT[b]])
                P.bfree(bk)
                return None
            def s4a():
                bhq = P.balloc()
                for p in range(3):
                    mm_fm(bhq, p * 128, hT[b], b_hT[b], C_HQ + p * 128)
                bhf = P.balloc()
                for p in range(3):
                    mm_fm(bhf, p * 128, hT[b], b_hT[b], C_HF + p * 128)
                bgl = P.balloc()
                for p in range(2):
                    mm_fm(bgl, p * 128, hT[b], b_hT[b], C_GQ + p * 128)
                for p in range(2):
                    mm_fm(bgl, 256 + p * 128, hT[b], b_hT[b], C_GK + p * 128)
                bga = P.balloc()
                mm_fm(bga, 0, hT[b], b_hT[b], C_GA, m=16)
                op("act", lambda e: e.activation(ff[:].rearrange("p a t -> p (a t)"), P.bank(bhf)[:, 0:384], AF.Exp, scale=-1.0),
                   reads=[BB[bhf]], writes=[b_ff])
                op("act", lambda e: e.activation(L1[:], ff[:], AF.Ln, bias=1.0), reads=[b_ff], writes=[b_L1])
                if l == 0:
                    op("dve", lambda e: e.tensor_scalar(lf[:, 0:3, :], L1[:], -1.0, -80.0, ALU.mult, ALU.max), reads=[b_L1], writes=[b_lf])
                else:
                    for p in range(3):
                        op("act", lambda e: e.activation(ff[:, p, :], ff[:, p, :], AF.Ln, bias=1.0, scale=lbT[:, 0, p:p + 1]),
                           reads=[b_ff, b_lb], writes=[b_ff])
                    op("dve", lambda e: e.tensor_tensor(lf[:, 0:3, :], ff[:], L1[:], ALU.subtract), reads=[b_ff, b_L1], writes=[b_lf])
                    op("dve", lambda e: e.tensor_scalar(lf[:, 0:3, :], lf[:, 0:3, :], -80.0, None, ALU.max), reads=[b_lf], writes=[b_lf])
                op("dve", lambda e: e.tensor_tensor(L1[:].rearrange("p a t -> p (a t)"), P.bank(bhf)[:, 0:384],
                                                    L1[:].rearrange("p a t -> p (a t)"), ALU.add),
                   reads=[BB[bhf], b_L1, b_lf], writes=[b_L1])
                P.bfree(bhf)
                op("act", lambda e: e.activation(silq[:].rearrange("p a t -> p (a t)"), P.bank(bhq)[:, 0:384], AF.Exp, scale=-1.0),
                   reads=[BB[bhq]], writes=[b_silq])
                op("act", lambda e: e.activation(silq[:], silq[:], AF.Ln, bias=1.0), reads=[b_silq], writes=[b_silq])
                op("act", lambda e: e.copy(gaT[:], P.bank(bga)[0:16, 0:128]), reads=[BB[bga]], writes=[b_gaT])
                for p in range(2):
                    op("pe", lambda e: e.matmul(P.bank(bga)[:, 128 + p * 128:256 + p * 128], wa2p[:, p * 128:(p + 1) * 128], gaT[:], start=True, stop=True),
                       reads=[b_wa2, b_gaT], writes=[BB[bga]])
                for p in range(2):
                    op("act", lambda e: e.activation(exg[:, p, :], P.bank(bga)[:, 128 + p * 128:256 + p * 128], AF.Exp, bias=nba[:, p:p + 1], scale=-1.0),
                       reads=[BB[bga], b_nba], writes=[b_exg])
                P.bfree(bga)
                op("act", lambda e: e.activation(exg[:], exg[:], AF.Ln, bias=1.0), reads=[b_exg], writes=[b_exg])
                op("dve", lambda e: e.tensor_scalar(lf[:, 3:5, :], exg[:], -1.0 / 16.0, -80.0, ALU.mult, ALU.max), reads=[b_exg], writes=[b_lf])
                for p in range(5):
                    init = zeros[:, 0:1] if t == 0 else Gc[pb][:, p, 127:128]
                    rd = [b_ones, b_lf, b_zeros] if t == 0 else [b_ones, b_lf, b_G[pb]]
                    op("dve", lambda e: e.tensor_tensor_scan(Gc[b][:, p, :], ones[:], lf[:, p, :], init, ALU.mult, ALU.add),
                       reads=rd, writes=[b_G[b]])
                G4 = Gc[b][:].rearrange("p a (c s) -> p a c s", c=2)
                E4 = Ee[:].rearrange("p a (c s) -> p a c s", c=2)
                op("dve", lambda e: e.tensor_tensor(E4, G4, G4[:, :, :, 31:32].broadcast_to([128, 5, 2, 64]), ALU.subtract),
                   reads=[b_G[b]], writes=[b_E])
                op("dve", lambda e: e.tensor_scalar(Ee[:], Ee[:], 80.0, -80.0, ALU.min, ALU.max), reads=[b_E], writes=[b_E])
                op("dve", lambda e: e.tensor_tensor(silq[:], Ee[:, 0:3, :], silq[:], ALU.subtract), reads=[b_E, b_silq], writes=[b_silq])
                op("dve", lambda e: e.tensor_tensor(L1[:], L1[:], Ee[:, 0:3, :], ALU.add), reads=[b_E, b_L1], writes=[b_L1])
                op("act", lambda e: e.activation(qfac[:, 0:3, :], silq[:], AF.Exp), reads=[b_silq], writes=[b_qf])
                if l == 0:
                    op("act", lambda e: e.activation(QKT[b][:, 1, 3:6, :], L1[:], AF.Exp, scale=-1.0), reads=[b_L1], writes=[b_QKT[b]])
                else:
                    for p in range(3):
                        op("act", lambda e: e.activation(QKT[b][:, 1, 3 + p, :], L1[:, p, :], AF.Exp, scale=-1.0, bias=lbT[:, 1, p:p + 1]),
                           reads=[b_L1, b_lb], writes=[b_QKT[b]])
                op("act", lambda e: e.activation(qfac[:, 3:5, :], Ee[:, 3:5, :], AF.Exp), reads=[b_E], writes=[b_qf])
                op("act", lambda e: e.activation(kfac[:, 3:5, :], Ee[:, 3:5, :], AF.Exp, scale=-1.0), reads=[b_E], writes=[b_kf])
                prev_mid = zeros[:, 0:5] if t == 0 else Gc[pb][:, :, 95]
                rdp = [b_zeros] if t == 0 else [b_G[pb]]
                op("dve", lambda e: e.tensor_tensor(dd[:, :, 0], Gc[b][:, :, 31], prev_mid, ALU.subtract), reads=[b_G[b]] + rdp, writes=[b_dd])
                op("dve", lambda e: e.tensor_tensor(dd[:, :, 1], Gc[b][:, :, 95], Gc[b][:, :, 31], ALU.subtract), reads=[b_G[b], b_dd], writes=[b_dd])
                op("act", lambda e: e.activation(cvec[b][:, 3:8, :], dd[:], AF.Exp), reads=[b_dd], writes=[b_cvec[b]])
                op("dve", lambda e: e.scalar_tensor_tensor(QKT[b][:, 0, 3:6, :], P.bank(bhq)[:, 0:384].rearrange("p (a t) -> p a t", a=3),
                                                           0.125, qfac[:, 0:3, :], ALU.mult, ALU.mult),
                   reads=[BB[bhq], b_qf], writes=[b_QKT[b]])
                P.bfree(bhq)
                op("dve", lambda e: e.scalar_tensor_tensor(QKT[b][:, 0, 6:8, :], P.bank(bgl)[:, 0:256].rearrange("p (a t) -> p a t", a=2),
                                                           32.0 ** -0.5, qfac[:, 3:5, :], ALU.mult, ALU.mult),
                   reads=[BB[bgl], b_qf], writes=[b_QKT[b]])
                op("dve", lambda e: e.tensor_tensor(QKT[b][:, 1, 6:8, :], P.bank(bgl)[:, 256:512].rearrange("p (a t) -> p a t", a=2),
                                                    kfac[:, 3:5, :], ALU.mult),
                   reads=[BB[bgl], b_kf], writes=[b_QKT[b]])
                P.bfree(bgl)
                bk = P.balloc()
                for j in range(5):
                    op("pe", lambda e: e.transpose(P.bank_bf(bk)[:, j * 128:(j + 1) * 128], QKT[b][:, 1, 3 + j, :], ident[:]),
                       reads=[b_QKT[b], b_ident], writes=[BB[bk]])
                op("act", lambda e: e.copy(QK[b][:, 384 + 384:384 + 1024], P.bank_bf(bk)[:, 0:640]), reads=[BB[bk]], writes=[b_QK[b]])
                P.bfree(bk)
                return None
            def s4b():
                for hf in range(2):
                    bk = P.balloc(); mm_tm(bk, hT[b], b_hT[b], C_V + hf * 512, 512)
                    op("act", lambda e: e.copy(Vall[b][:, hf * 512:(hf + 1) * 512], P.bank(bk)), reads=[BB[bk]], writes=[b_V[b]])
                    P.bfree(bk)
                for hf in range(2):
                    bk = P.balloc(); mm_tm(bk, hT[b], b_hT[b], C_G + hf * 512, 512)
                    op("act", lambda e: e.activation(GG[b][:, hf * 512:(hf + 1) * 512], P.bank(bk), AF.Silu), reads=[BB[bk]], writes=[b_GG[b]])
                    P.bfree(bk)
                op("pool", lambda e: e.tensor_tensor(GG[b][:], GG[b][:], gain[:], ALU.mult), reads=[b_GG[b], b_gain], writes=[b_GG[b]])
                return None
            def s5():
                kTok = QK[b][:, 384:384 + 1024].rearrange("p (a d) -> p a d", a=8)
                for c in range(2):
                    bu = [P.balloc(), P.balloc()]
                    for p in range(8):
                        op("pe", lambda e: e.matmul(P.bank(bu[p // 4])[:, (p % 4) * 128:(p % 4 + 1) * 128],
                                                    kTok[64 * c:64 * c + 64, p, :], Vall[b][64 * c:64 * c + 64, p * 128:(p + 1) * 128],
                                                    start=True, stop=True),
                           reads=[b_QK[b], b_V[b]], writes=[BB[bu[p // 4]]])
                    op("dve", lambda e: e.tensor_tensor(tmpW, Wst[:], cvec[b][:, :, c:c + 1].broadcast_to([128, 8, 128]), ALU.mult),
                       reads=[b_W, b_cvec[b]], writes=[b_tW])
                    op("act", lambda e: e.copy(Sbf[:, c, :, :], tmpW), reads=[b_tW], writes=[b_Sbf[c]])
                    for g in range(2):
                        op("dve", lambda e: e.tensor_tensor(Wst[:, 4 * g:4 * g + 4, :].rearrange("p a d -> p (a d)"),
                                                            tmpW[:, 4 * g:4 * g + 4, :].rearrange("p a d -> p (a d)"), P.bank(bu[g]), ALU.add),
                           reads=[b_tW, BB[bu[g]]], writes=[b_W])
                    P.bfree(bu[0]); P.bfree(bu[1])
                return None
            def s6():
                for half in range(2):
                    for g in range(2):
                        bk = P.balloc()
                        for hh in range(4):
                            h = 8 * g + 2 * hh + half
                            p = h // 2
                            op("pe", lambda e: e.matmul(P.bank(bk)[:, hh * 128:(hh + 1) * 128],
                                                        QKT[b][64 * half:64 * half + 64, 1, p, :], QKT[b][64 * half:64 * half + 64, 0, p, :],
                                                        start=True, stop=True),
                               reads=[b_QKT[b]], writes=[BB[bk]])
                        i0 = half * 8 + g * 4
                        op("dve", lambda e: e.copy_predicated(PT[b][:, i0:i0 + 4, :].rearrange("p a t -> p (a t)"),
                                                              mask4[:].rearrange("p a t -> p (a t)"), P.bank(bk)),
                           reads=[BB[bk], b_mask, b_PT[b]], writes=[b_PT[b]])
                        P.bfree(bk)
                return None
            def s7():
                bo = [P.balloc(), P.balloc()]
                for half in range(2):
                    for j in range(8):
                        h = 2 * j + half
                        pi = (h % 2) * 8 + (h // 8) * 4 + (h % 8) // 2
                        op("pe", lambda e: e.matmul(P.bank(bo[half])[:, j * 64:j * 64 + 64], PT[b][:, pi, :], Vall[b][:, h * 64:(h + 1) * 64],
                                                    start=(j == 0), stop=False),
                           reads=[b_PT[b], b_V[b]], writes=[BB[bo[half]]])
                for half in range(2):
                    for j in range(8):
                        h = 2 * j + half
                        p = h // 2
                        for c in range(2):
                            op("pe", lambda e: e.matmul(P.bank(bo[half])[64 * c:64 * c + 64, j * 64:j * 64 + 64],
                                                        QKT[b][64 * half:64 * half + 64, 0, p, 64 * c:64 * c + 64],
                                                        Sbf[64 * half:64 * half + 64, c, p, 64 * half:64 * half + 64],
                                                        start=False, stop=(j == 7)),
                               reads=[b_QKT[b], b_Sbf[c]], writes=[BB[bo[half]]])
                ov4 = o_sb[:].rearrange("p (j two e) -> p j two e", two=2, e=64)
                for half in range(2):
                    op("act", lambda e: e.copy(ov4[:, :, half, :], P.bank(bo[half]).rearrange("p (j e) -> p j e", e=64)),
                       reads=[BB[bo[half]]], writes=[b_osb])
                    P.bfree(bo[half])
                op("act", lambda e: e.activation(sq[:], o_sb[:], AF.Square), reads=[b_osb], writes=[b_sq])
                op("dve", lambda e: e.tensor_reduce(hs[:, 0, :], sq[:].rearrange("p (h e) -> p h e", e=64), AX.X, ALU.add), reads=[b_sq], writes=[b_hs])
                op("act", lambda e: e.activation(hs[:, 1, :], hs[:, 0, :], AF.Ln, bias=epsc[:, 0:1], scale=1.0 / 64.0), reads=[b_hs], writes=[b_hs])
                op("act", lambda e: e.activation(hs[:, 1, :], hs[:, 1, :], AF.Exp, scale=-0.5), reads=[b_hs], writes=[b_hs])
                op("dve", lambda e: e.tensor_tensor(sq[:].rearrange("p (h e) -> p h e", e=64), o_sb[:].rearrange("p (h e) -> p h e", e=64),
                                                    hs[:, 1, :].unsqueeze(2).broadcast_to([128, 16, 64]), ALU.mult),
                   reads=[b_osb, b_hs, b_sq], writes=[b_sq])
                op("dve", lambda e: e.tensor_tensor(merged[:], sq[:], GG[b][:], ALU.mult), reads=[b_sq, b_GG[b]], writes=[b_mg])
                return None
            def s8():
                transpose8(merged, b_mg, mT, b_mT)
                for hf in range(2):
                    bk = P.balloc()
                    for k in range(8):
                        op("pe", lambda e: e.matmul(P.bank(bk), mT[:, k, :], w_out[:, k, hf * 512:(hf + 1) * 512], start=(k == 0), stop=(k == 7)),
                           reads=[b_mT, b_wout], writes=[BB[bk]])
                    op("dve", lambda e: e.tensor_tensor(hn[:, hf * 512:(hf + 1) * 512], P.bank(bk), gate_m[:, hf * 512:(hf + 1) * 512], ALU.mult),
                       reads=[BB[bk], b_mod[2]], writes=[b_hn])
                    P.bfree(bk)
                op("dve", lambda e: e.tensor_tensor(x1[b][:], hn[:], xt[b][:], ALU.add), reads=[b_hn, b_xt[b]], writes=[b_x1[b]])
                P.dma("sp", xs_d[t * 128:(t + 1) * 128, :], x1[b][:], reads=[b_x1[b]], writes=[xs_bufs[t]])
                return None
            def s9():
                rmsnorm_to_bf(x1[b], b_x1[b], st[b], b_st[b], 2, A_f, b_mod[4], shift_f, b_mod[3], False)
                transpose8(h_bf, b_hbf, h2T[b], b_h2T[b])
                P.dma("sp", h2T_d[:, :, t * 128:(t + 1) * 128], h2T[b][:], reads=[b_h2T[b]], writes=[h2T_bufs[t]])
                bk = P.balloc()
                for k in range(8):
                    op("pe", lambda e: e.matmul(P.bank(bk)[:, 0:36], h2T[b][:, k, :], Wr[:, k, :], start=(k == 0), stop=(k == 7)),
                       reads=[b_h2T[b], b_Wr], writes=[BB[bk]])
                op("dve", lambda e: e.tensor_tensor(rl[:], P.bank(bk)[:, 0:36], rbias[:], ALU.add), reads=[BB[bk], b_rbias], writes=[b_rl])
                P.bfree(bk)
                R = [b_rs]
                op("dve", lambda e: e.tensor_reduce(rs[:, 0:1], rl[:, 0:4], AX.X, ALU.max, negate=True), reads=[b_rl], writes=R)
                op("dve", lambda e: e.tensor_scalar(rs[:, 8:12], rl[:, 0:4], rs[:, 0:1], 0.0, ALU.add, ALU.is_ge), reads=[b_rl] + R, writes=R)
                op("act", lambda e: e.activation(rs[:, 12:16], rl[:, 0:4], AF.Exp, bias=rs[:, 0:1], scale=1.0, accum_out=rs[:, 1:2]),
                   reads=[b_rl] + R, writes=R)
                op("dve", lambda e: e.reciprocal(rs[:, 2:3], rs[:, 1:2]), reads=R, writes=R)
                op("dve", lambda e: e.tensor_scalar(rs[:, 8:12], rs[:, 8:12], -1.0, 1e30, ALU.add, ALU.mult), reads=R, writes=R)
                op("dve", lambda e: e.tensor_tensor(rm[:, 0, :].rearrange("p (g x) -> p g x", g=4), rl[:, 4:36].rearrange("p (g x) -> p g x", g=4),
                                                    rs[:, 8:12].unsqueeze(2).broadcast_to([128, 4, 8]), ALU.add),
                   reads=[b_rl] + R, writes=[b_rm])
                op("dve", lambda e: e.max(top8[:], rm[:, 0, :]), reads=[b_rm], writes=[b_top8])
                op("dve", lambda e: e.tensor_scalar(rm[:, 1, :], rm[:, 0, :], top8[:, 0:1], None, ALU.is_equal), reads=[b_rm, b_top8], writes=[b_rm])
                op("dve", lambda e: e.tensor_scalar(rm[:, 2, :], rm[:, 0, :], top8[:, 1:2], None, ALU.is_equal), reads=[b_rm, b_top8], writes=[b_rm])
                op("dve", lambda e: e.tensor_tensor(rs[:, 3:4], top8[:, 1:2], top8[:, 0:1], ALU.subtract), reads=[b_top8] + R, writes=R)
                op("act", lambda e: e.activation(rs[:, 4:5], rs[:, 3:4], AF.Exp), reads=R, writes=R)
                op("dve", lambda e: e.tensor_scalar(rs[:, 5:6], rs[:, 4:5], 1.0, None, ALU.add), reads=R, writes=R)
                op("dve", lambda e: e.reciprocal(rs[:, 5:6], rs[:, 5:6]), reads=R, writes=R)
                op("dve", lambda e: e.tensor_tensor(rs[:, 6:7], rs[:, 5:6], rs[:, 2:3], ALU.mult), reads=R, writes=R)
                op("dve", lambda e: e.tensor_tensor(rs[:, 7:8], rs[:, 6:7], rs[:, 4:5], ALU.mult), reads=R, writes=R)
                op("dve", lambda e: e.tensor_scalar(comb[:, t, :], rm[:, 1, :], rs[:, 6:7], None, ALU.mult), reads=[b_rm] + R, writes=[b_comb[t]])
                op("dve", lambda e: e.scalar_tensor_tensor(comb[:, t, :], rm[:, 2, :], rs[:, 7:8], comb[:, t, :], ALU.mult, ALU.add),
                   reads=[b_rm, b_comb[t]] + R, writes=[b_comb[t]])
                return None
            return dict(s12=s12, s3=s3, s4a=s4a, s4b=s4b, s5=s5, s6=s6, s7=s7, s8=s8, s9=s9)

        P.recording = SCHED
        st_prev = None
        for t in range(n_tiles + 1):
            cur = make_stages(t) if t < n_tiles else None
            seq = [(st_prev, "s5"), (cur, "s12"), (st_prev, "s6"), (cur, "s3"), (st_prev, "s7"), (cur, "s4a"),
                   (st_prev, "s8"), (cur, "s4b"), (st_prev, "s9")]
            for d_, k_ in seq:
                if d_ is not None:
                    d_[k_]()
            st_prev = cur
        if dbg and l == 0:
            P.dma("sp", comb_d, comb[:], reads=b_comb, writes=[])
        P.barrier()
        esA.close()

        if not do_moe:
            return
        esB = contextlib.ExitStack(); P.stacks.append(esB)
        NPASS = 2
        TPP = NT // NPASS
        h2ps = [sb("h2p%d" % i, [128, 8, TPP * 128], BF16, esB) for i in range(2)]; b_h2ps = [Buf("h2p0"), Buf("h2p1")]
        acc = sb("acc", [128, TPP, D], F32, esB); b_acc = [Buf("acc%d" % i) for i in range(TPP)]
        ewg = [sb("ewg%d" % i, [128, 8, DE], BF16, esB) for i in range(2)]
        ewu = [sb("ewu%d" % i, [128, 8, DE], BF16, esB) for i in range(2)]
        ewd = [sb("ewd%d" % i, [128, 2, D], BF16, esB) for i in range(2)]
        b_ew = [Buf("ew0"), Buf("ew1")]
        sgt = [sb("sgt%d" % i, [128, 512], BF16, esB) for i in range(2)]; b_sgt = [Buf("sgt0"), Buf("sgt1")]
        hid = [sb("hid%d" % i, [128, 2, 512], BF16, esB) for i in range(2)]; b_hid = [Buf("hid0"), Buf("hid1")]
        xr = [sb("xr%d" % i, [128, D], F32, esB) for i in range(2)]; b_xr = [Buf("xr0"), Buf("xr1")]
        stB = [sb("stB%d" % i, [128, 2], F32, esB) for i in range(2)]; b_stB = [Buf("stB0"), Buf("stB1")]
        junkB = sb("junkB", [128, D], BF16, esB); b_junkB = Buf("junkB")
        nfin = sb("nfin", [128, D], F32, esB); b_nfin = Buf("nfin")
        if last:
            P.dma("sp", nfin[:], nfin_d.partition_broadcast(128), writes=[b_nfin])
        BB = P.bank_bufs

        def load_expert(e_idx, slot):
            P.dma("pool", ewg[slot][:], ewg_d[l, e_idx].rearrange("(k p) f -> p k f", p=128), writes=[b_ew[slot]])
            P.dma("pool", ewu[slot][:], ewu_d[l, e_idx].rearrange("(k p) f -> p k f", p=128), writes=[b_ew[slot]])
            P.dma("pool", ewd[slot][:], ewd_d[l, e_idx].rearrange("(k p) f -> p k f", p=128), writes=[b_ew[slot]])

        P.recording = SCHED
        for pa in range(NPASS):
            h2p, b_h2p = h2ps[pa % 2], b_h2ps[pa % 2]
            P.dma("sp", h2p[:], h2T_d[:, :, pa * TPP * 128:(pa + 1) * TPP * 128],
                  reads=h2T_bufs[pa * TPP:(pa + 1) * TPP], writes=[b_h2p])
            load_expert(0, 0)
            pending = None
            blk = 0
            for ex in range(n_exp):
                slot = ex % 2
                for tb in range(TPP // 4):
                    hb = blk % 2
                    blk += 1
                    for fc in range(2):
                        bg = P.balloc()
                        for k in range(8):
                            op("pe", lambda e: e.matmul(P.bank(bg), ewg[slot][:, k, fc * 128:(fc + 1) * 128], h2p[:, k, tb * 512:(tb + 1) * 512],
                                                        start=(k == 0), stop=(k == 7)),
                               reads=[b_ew[slot], b_h2p], writes=[BB[bg]])
                        bu = P.balloc()
                        for k in range(8):
                            op("pe", lambda e: e.matmul(P.bank(bu), ewu[slot][:, k, fc * 128:(fc + 1) * 128], h2p[:, k, tb * 512:(tb + 1) * 512],
                                                        start=(k == 0), stop=(k == 7)),
                               reads=[b_ew[slot], b_h2p], writes=[BB[bu]])
                        op("act", lambda e: e.activation(sgt[fc][:], P.bank(bg), AF.Silu), reads=[BB[bg]], writes=[b_sgt[fc]])
                        P.bfree(bg)
                        op("dve", lambda e: e.tensor_tensor(hid[hb][:, fc, :], sgt[fc][:], P.bank(bu), ALU.mult),
                           reads=[b_sgt[fc], BB[bu]], writes=[b_hid[hb]])
                        P.bfree(bu)
                    if pending is not None:
                        pending()
                    if tb == 0 and ex + 1 < n_exp:
                        load_expert(ex + 1, 1 - slot)

                    def down(ex=ex, slot=slot, tb=tb, hb=hb):
                        for tt in range(4):
                            ti = tb * 4 + tt
                            for dh in range(2):
                                bd = P.balloc()
                                for fc in range(2):
                                    op("pe", lambda e: e.matmul(P.bank(bd), hid[hb][:, fc, tt * 128:(tt + 1) * 128],
                                                                ewd[slot][:, fc, dh * 512:(dh + 1) * 512], start=(fc == 0), stop=(fc == 1)),
                                       reads=[b_hid[hb], b_ew[slot]], writes=[BB[bd]])
                                cw = comb[:, pa * TPP + ti, ex:ex + 1]
                                a_ = acc[:, ti, dh * 512:(dh + 1) * 512]
                                if ex == 0:
                                    op("dve", lambda e: e.tensor_scalar(a_, P.bank(bd), cw, None, ALU.mult),
                                       reads=[BB[bd], b_comb[pa * TPP + ti]], writes=[b_acc[ti]])
                                else:
                                    op("dve", lambda e: e.scalar_tensor_tensor(a_, P.bank(bd), cw, a_, ALU.mult, ALU.add),
                                       reads=[BB[bd], b_comb[pa * TPP + ti], b_acc[ti]], writes=[b_acc[ti]])
                                P.bfree(bd)
                    pending = down
            pending()
            for ti in range(TPP):
                t = pa * TPP + ti
                b = ti % 2
                P.dma("sp", xr[b][:], xs_d[t * 128:(t + 1) * 128, :], reads=[xs_bufs[t]], writes=[b_xr[b]])
                a_t = acc[:, ti, :]
                op("dve", lambda e: e.tensor_tensor(a_t, a_t, gate_f, ALU.mult), reads=[b_acc[ti], b_mod[5]], writes=[b_acc[ti]])
                op("dve", lambda e: e.tensor_tensor(xr[b][:], a_t, xr[b][:], ALU.add), reads=[b_acc[ti], b_xr[b]], writes=[b_xr[b]])
                if not last:
                    P.dma("sp", xs_d[t * 128:(t + 1) * 128, :], xr[b][:], reads=[b_xr[b]], writes=[xs_bufs[t]])
                else:
                    op("act", lambda e: e.activation(junkB[:], xr[b][:], AF.Square, accum_out=stB[b][:, 0:1]),
                       reads=[b_xr[b]], writes=[b_junkB, b_stB[b]])
                    op("act", lambda e: e.activation(stB[b][:, 1:2], stB[b][:, 0:1], AF.Ln, bias=epsc[:, 0:1], scale=1.0 / D),
                       reads=[b_stB[b]], writes=[b_stB[b]])
                    op("act", lambda e: e.activation(stB[b][:, 1:2], stB[b][:, 1:2], AF.Exp, scale=-0.5), reads=[b_stB[b]], writes=[b_stB[b]])
                    op("dve", lambda e: e.scalar_tensor_tensor(a_t, xr[b][:], stB[b][:, 1:2], nfin[:], ALU.mult, ALU.mult),
                       reads=[b_xr[b], b_stB[b], b_nfin, b_acc[ti]], writes=[b_acc[ti]])
                    P.dma("sp", out_d[t * 128:(t + 1) * 128, :], a_t, reads=[b_acc[ti]], writes=[out_bufs[t]])
        P.barrier()
        esB.close()

    try:
        for l in range(n_layers):
            layer(l, last=(l == n_layers - 1))
    except _Stop:
        for es_ in reversed(P.stacks):
            es_.close()
    P.finish("sp")
    P.barrier()
    P.close()
    return P


_CACHE = {}


def _in_maps(inputs):
    consts = _host_consts()
    shared = {}
    for k in ["w_ada", "b_ada", "norm_mix", "norm_ffn", "w_in", "ret_norm", "hgrn_norm", "hgrn_lb_logits", "gla_wa2", "gla_ba",
              "gla_norm", "w_out", "router_group_w", "router_group_b", "router_expert_w", "router_expert_b",
              "expert_w_gate", "expert_w_up", "expert_w_down", "norm_final"]:
        shared[k] = np.ascontiguousarray(np.asarray(inputs[k], dtype=np.float32))
    shared.update(consts)
    lbl = shared["hgrn_lb_logits"].reshape(DEPTH, 3, 128).transpose(0, 2, 1)
    shared["hgrn_lb_logits"] = np.ascontiguousarray(lbl)
    wa2 = shared["gla_wa2"]
    wa2p = np.zeros((DEPTH, 16, 256), np.float32)
    ba = shared["gla_ba"]
    bap = np.zeros((DEPTH, 128, 2), np.float32)
    for h in range(4):
        wa2p[:, :, h * 64:h * 64 + 32] = wa2[:, :, h * 32:(h + 1) * 32]
        pair, half = h // 2, h % 2
        bap[:, half * 64:half * 64 + 32, pair] = ba[:, h * 32:(h + 1) * 32]
    shared["gla_wa2"] = wa2p
    shared["gla_ba"] = bap
    x = np.asarray(inputs["x"], dtype=np.float32)
    c = np.asarray(inputs["c"], dtype=np.float32)
    pos = np.asarray(inputs["positions"], dtype=np.int32)
    maps = []
    for b in range(8):
        m = dict(shared)
        m["x"] = np.ascontiguousarray(x[b])
        m["c"] = np.ascontiguousarray(c[b].reshape(8, 128).T)
        m["pos"] = np.ascontiguousarray(pos[b].reshape(NT, 128).T)
        maps.append(m)
    return maps


def kernel(**inputs):
    if "prog" not in _CACHE:
        _CACHE["prog"] = build()
    P = _CACHE["prog"]
    maps = _in_maps(inputs)
    res = run_bass_kernel_spmd(P.nc, maps, core_ids=list(range(8)))
    out = np.stack([np.asarray(r["out"], dtype=np.float32) for r in res.results], axis=0)
    return out
```
